# Optimizing a Trainium2 kernel written in Bass

```python
import jax, jax.numpy as jnp
from jax import lax
import numpy as np

D_MODEL = 1024
BATCH = 8
SEQ = 4096
DEPTH = 1

RET_HEADS = 8
RET_DK = 64
RET_DV = 128
RET_CHUNK = 128
ATT_GROUPS = ((128, 1), (512, 4), (2048, 16))
ATT_HEADS_PER_GROUP = 4
ATT_HEADS = 12
ATT_DH = 128
ATT_BLOCK = 128
PEER_HEADS = 8
PEER_NKEYS = 128
PEER_EXPERTS = PEER_NKEYS * PEER_NKEYS
PEER_DKEY = 256
PEER_TOPK = 16
PEER_TOKEN_BLOCK = 128
EPS = 1e-6

RET_QK_W = RET_HEADS * RET_DK
RET_V_W = RET_HEADS * RET_DV
ATT_W = ATT_HEADS * ATT_DH
ATT_OUT_W = ATT_HEADS_PER_GROUP * ATT_DH
IN_SPLITS = (RET_QK_W, RET_QK_W, RET_V_W, RET_V_W, ATT_W, ATT_W, ATT_W, D_MODEL, D_MODEL)
IN_W = 2 * RET_QK_W + 2 * RET_V_W + 3 * ATT_W + 2 * D_MODEL

kernel_name = "hybrid_retention_dilated_attn_peer"


def rmsnorm(x, g):
    xf = x.astype(jnp.float32)
    y = xf * lax.rsqrt(jnp.mean(xf * xf, axis=-1, keepdims=True) + EPS)
    return (y * g.astype(jnp.float32)).astype(x.dtype)


def head_norm(x):
    xf = x.astype(jnp.float32)
    return (xf * lax.rsqrt(jnp.mean(xf * xf, axis=-1, keepdims=True) + EPS)).astype(x.dtype)


def split_points():
    pts, acc = [], 0
    for w in IN_SPLITS[:-1]:
        acc += w
        pts.append(acc)
    return pts


def retention(q, k, v):
    B, S, H, DK = q.shape
    DV = v.shape[-1]
    C = RET_CHUNK
    n = S // C
    dt = q.dtype
    log_g = jnp.log1p(-jnp.exp2(-5.0 - jnp.arange(H, dtype=jnp.float32)))
    pos = jnp.arange(C, dtype=jnp.float32)
    q = q.reshape(B, n, C, H, DK)
    k = (k * (DK ** -0.5)).reshape(B, n, C, H, DK)
    v = v.reshape(B, n, C, H, DV)
    diff = pos[:, None] - pos[None, :]
    decay = jnp.where(diff[None] >= 0, jnp.exp(jnp.maximum(diff, 0.0)[None] * log_g[:, None, None]), 0.0)
    qk = jnp.einsum('bnihd,bnjhd->bnhij', q, k) * decay.astype(dt)
    inner = jnp.einsum('bnhij,bnjhe->bnihe', qk, v)
    w_k = jnp.exp((C - 1 - pos)[:, None] * log_g[None]).astype(dt)
    kv = jnp.einsum('bnjhd,bnjhe->nbhde', k * w_k[:, :, None], v)
    chunk_decay = jnp.exp(C * log_g).astype(dt)[None, :, None, None]

    def step(state, kv_c):
        return chunk_decay * state + kv_c, state

    _, r_prev = lax.scan(step, jnp.zeros((B, H, DK, DV), dt), kv)
    w_q = jnp.exp((pos + 1)[:, None] * log_g[None]).astype(dt)
    cross = jnp.einsum('bnihd,nbhde->bnihe', q * w_q[:, :, None], r_prev)
    return (inner + cross).reshape(B, S, H, DV)


def alibi_slopes(n_heads):
    return jnp.exp2(-8.0 * jnp.arange(1, n_heads + 1, dtype=jnp.float32) / n_heads)


def dilated_group(q, k, v, window, dilation, slopes):
    B, S, H, Dh = q.shape
    dt = q.dtype
    L = S // dilation
    n = -(-L // ATT_BLOCK)
    Lp = n * ATT_BLOCK
    span = window // dilation

    def strided(t):
        t = t.reshape(B, L, dilation, H, Dh).transpose(0, 2, 1, 3, 4)
        t = jnp.pad(t, ((0, 0), (0, 0), (0, Lp - L), (0, 0), (0, 0)))
        return t.reshape(B, dilation, n, ATT_BLOCK, H, Dh)

    def with_prev(t):
        prev = jnp.concatenate([jnp.zeros_like(t[:, :, :1]), t[:, :, :-1]], axis=2)
        return jnp.concatenate([prev, t], axis=3)

    qb = strided(q)
    kk = with_prev(strided(k))
    vv = with_prev(strided(v))
    s = jnp.einsum('brnqhd,brnkhd->brnhqk', qb, kk).astype(jnp.float32) * (Dh ** -0.5)
    iq = jnp.arange(ATT_BLOCK)[:, None]
    jk = jnp.arange(2 * ATT_BLOCK)[None, :]
    dist = iq + ATT_BLOCK - jk
    blk = jnp.arange(n)[:, None, None]
    valid = (dist >= 0) & (dist <= span) & (blk * ATT_BLOCK + jk - ATT_BLOCK >= 0)
    bias = -(slopes.astype(jnp.float32)[:, None, None] * (dilation * dist).astype(jnp.float32))
    s = jnp.where(valid[:, None], s + bias, -jnp.inf)
    m = jnp.max(s, axis=-1)
    p = jnp.exp(s - m[..., None])
    den = jnp.sum(p, axis=-1)
    o = jnp.einsum('brnhqk,brnkhd->brnqhd', (p / den[..., None]).astype(dt), vv)

    def unstride(t):
        t = t.reshape(B, dilation, Lp, *t.shape[4:])[:, :, :L]
        t = jnp.moveaxis(t, 1, 2)
        return t.reshape(B, S, *t.shape[3:])

    return (unstride(o), unstride(jnp.moveaxis(m, 3, 4)), unstride(jnp.moveaxis(den, 3, 4)))


def dilated_attention(q, k, v):
    B, S = q.shape[:2]
    dt = q.dtype
    slopes = alibi_slopes(ATT_HEADS)
    outs, maxs, dens = [], [], []
    for g, (w, d) in enumerate(ATT_GROUPS):
        sl = slice(g * ATT_HEADS_PER_GROUP, (g + 1) * ATT_HEADS_PER_GROUP)
        o, m, den = dilated_group(q[:, :, sl], k[:, :, sl], v[:, :, sl], w, d, slopes[sl])
        outs.append(o)
        maxs.append(m)
        dens.append(den)
    m_all = jnp.stack(maxs)
    wts = jnp.stack(dens) * jnp.exp(m_all - jnp.max(m_all, axis=0))
    wts = wts / jnp.sum(wts, axis=0)
    o = jnp.einsum('gbsh,gbshd->bshd', wts.astype(dt), jnp.stack(outs))
    return o.reshape(B, S, ATT_OUT_W)


def peer(x, wq, subkeys, u_tab, v_tab):
    B, S, D = x.shape
    T = PEER_TOKEN_BLOCK
    K = PEER_TOPK
    xt = x.reshape(-1, T, D)

    def block(xb):
        q = (xb @ wq).reshape(T, PEER_HEADS, 2, PEER_DKEY // 2)
        sc = jnp.einsum('thpc,hpnc->thpn', q, subkeys).astype(jnp.float32)
        top_s, top_i = lax.top_k(sc, K)
        cand = (top_s[:, :, 0, :, None] + top_s[:, :, 1, None, :]).reshape(T, PEER_HEADS, K * K)
        best_s, best_c = lax.top_k(cand, K)
        ia = jnp.take_along_axis(top_i[:, :, 0], best_c // K, axis=-1)
        ib = jnp.take_along_axis(top_i[:, :, 1], best_c % K, axis=-1)
        expert = ia * PEER_NKEYS + ib
        gate = jax.nn.softmax(best_s, axis=-1).astype(xb.dtype)
        act = jax.nn.gelu(jnp.einsum('thkd,td->thk', u_tab[expert], xb), approximate=False)
        return jnp.einsum('thk,thkd->td', gate * act, v_tab[expert])

    return lax.map(block, xt).reshape(B, S, D)


def setup_inputs(seed: int = 0) -> dict:
    key = jax.random.key(seed)
    ks = jax.random.split(key, 13)
    f32 = jnp.float32
    nrm = lambda k, shape, scale: jax.random.normal(k, shape, f32) * scale
    return {
        "x": jax.random.normal(ks[0], (BATCH, SEQ, D_MODEL), f32),
        "norm1_g": 1.0 + nrm(ks[1], (DEPTH, D_MODEL), 0.02),
        "w_in": nrm(ks[2], (DEPTH, D_MODEL, IN_W), D_MODEL ** -0.5),
        "w_ret_out": nrm(ks[3], (DEPTH, RET_V_W, D_MODEL), RET_V_W ** -0.5),
        "w_att_out": nrm(ks[4], (DEPTH, ATT_OUT_W, D_MODEL), ATT_OUT_W ** -0.5),
        "w_out": nrm(ks[5], (DEPTH, D_MODEL, D_MODEL), D_MODEL ** -0.5),
        "norm2_g": 1.0 + nrm(ks[6], (DEPTH, D_MODEL), 0.02),
        "peer_wq": nrm(ks[7], (DEPTH, D_MODEL, PEER_HEADS * PEER_DKEY), D_MODEL ** -0.5),
        "peer_subkeys": nrm(ks[8], (DEPTH, PEER_HEADS, 2, PEER_NKEYS, PEER_DKEY // 2), (PEER_DKEY // 2) ** -0.5),
        "peer_u": nrm(ks[9], (DEPTH, PEER_EXPERTS, D_MODEL), D_MODEL ** -0.5),
        "peer_v": nrm(ks[10], (DEPTH, PEER_EXPERTS, D_MODEL), PEER_HEADS ** -0.5),
        "normf_g": 1.0 + nrm(ks[11], (D_MODEL,), 0.02),
    }


def reference(x, norm1_g, w_in, w_ret_out, w_att_out, w_out, norm2_g, peer_wq, peer_subkeys, peer_u, peer_v, normf_g):
    B, S, D = x.shape
    h = x
    for l in range(DEPTH):
        xn = rmsnorm(h, norm1_g[l])
        proj = xn @ w_in[l]
        rq, rk, rv, rg, aq, ak, av, gr, ga = jnp.split(proj, split_points(), axis=-1)
        ret = retention(rq.reshape(B, S, RET_HEADS, RET_DK), rk.reshape(B, S, RET_HEADS, RET_DK),
                        rv.reshape(B, S, RET_HEADS, RET_DV))
        ret = jax.nn.silu(rg) * head_norm(ret).reshape(B, S, RET_V_W)
        r_branch = ret @ w_ret_out[l]
        att = dilated_attention(aq.reshape(B, S, ATT_HEADS, ATT_DH), ak.reshape(B, S, ATT_HEADS, ATT_DH),
                                av.reshape(B, S, ATT_HEADS, ATT_DH))
        a_branch = att @ w_att_out[l]
        merged = jax.nn.sigmoid(gr) * r_branch + jax.nn.sigmoid(ga) * a_branch
        h = h + merged @ w_out[l]
        h = h + peer(rmsnorm(h, norm2_g[l]), peer_wq[l], peer_subkeys[l], peer_u[l], peer_v[l])
    return rmsnorm(h, normf_g)
```

```python
import numpy as np
from contextlib import ExitStack
import concourse.bass as bass
import concourse.mybir as mybir
from concourse.bass_utils import run_bass_kernel_spmd

F32 = mybir.dt.float32
BF16 = mybir.dt.bfloat16
U32 = mybir.dt.uint32
ALU = mybir.AluOpType
AF = mybir.ActivationFunctionType
AX = mybir.AxisListType

D = 1024
IN_W = 9728
EPS = 1e-6
NEG = -1e30


class Buf:
    __slots__ = ("name", "w", "r", "dsem", "dcount", "wl")

    def __init__(self, name, multi=False):
        self.name = name
        self.w = None
        self.r = []
        self.dsem = None
        self.dcount = 0
        self.wl = {} if multi else None


class Eng:
    def __init__(self, name, handle, sem):
        self.name = name
        self.h = handle
        self.sem = sem
        self.count = 0
        self.seen = {}


class FW:
    def __init__(self, nc, stack):
        self.nc = nc
        self.stack = stack
        self.engs = {}
        for name, h in (("pe", nc.tensor), ("act", nc.scalar), ("dve", nc.vector),
                        ("pool", nc.gpsimd), ("sp", nc.sync)):
            sem = stack.enter_context(nc.semaphore("sem_" + name))
            self.engs[name] = Eng(name, h, sem)
        self.nbuf = 0
        self.ninst = 0
        self.free_sems = []
        self.free_sems_sw = []
        self.dma_counts = {}
        self.no_barrier = set()

    def buf(self, name=None, dma=False, stack=None, multi=False, sw=False):
        self.nbuf += 1
        b = Buf(name or f"b{self.nbuf}", multi)
        if dma:
            pool = self.free_sems_sw if sw else self.free_sems
            if pool:
                b.dsem, b.dcount = pool.pop()
            else:
                b.dsem = self.stack.enter_context(self.nc.semaphore(f"ds{self.nbuf}"))
            if stack is not None:
                stack.callback(self._release, b, pool)
        return b

    def _release(self, b, pool):
        pool.append((b.dsem, b.dcount))

    def bufs(self, n, name, dma=False, stack=None, sw=False):
        return [self.buf(f"{name}{i}", dma, stack, sw=sw) for i in range(n)]

    def _waits(self, E, reads, writes, extra=()):
        waits = {}

        def need(ev):
            if ev is None:
                return
            sem, val, src = ev
            if src == "pe" and E.name == "pe":
                return
            k = id(sem)
            if E.seen.get(k, 0) >= val:
                return
            if src == E.name:
                val = E.count
            if k not in waits or waits[k][1] < val:
                waits[k] = (sem, val)

        for b in reads:
            if b.wl is not None:
                for ev in b.wl.values():
                    need(ev)
            else:
                need(b.w)
        for b in writes:
            if b.wl is not None:
                continue
            need(b.w)
            for ev in b.r:
                need(ev)
        for ev in extra:
            need(ev)
        for k, (sem, val) in waits.items():
            E.h.wait_ge(sem, val)
            E.seen[k] = val
            self.ninst += 1

    def op(self, eng, fn, reads=(), writes=(), inc=True):
        E = self.engs[eng]
        self._waits(E, reads, writes)
        ins = fn(E.h)
        self.ninst += 1
        if inc:
            E.count += 1
            ins.then_inc(E.sem, 1)
            ev = (E.sem, E.count, eng)
        else:
            assert eng == "pe"
            ev = (E.sem, E.count + 1, eng)
        self._record(ev, reads, writes)
        return ev

    def _record(self, ev, reads, writes):
        for b in reads:
            if b.wl is not None:
                continue
            b.r = [e for e in b.r if e[0] is not ev[0]]
            b.r.append(ev)
        for b in writes:
            if b.wl is not None:
                b.wl[id(ev[0])] = ev
            else:
                b.w = ev
                b.r = []

    def dma(self, eng, owner, fns, reads=(), writes=()):
        E = self.engs[eng]
        extra = []
        if owner.dcount > 0:
            extra.append((owner.dsem, owner.dcount, "dma"))
        self._waits(E, reads, writes, extra)
        for fn in fns:
            ins = fn(E.h)
            owner.dcount += 16
            ins.then_inc(owner.dsem, 16)
            self.ninst += 1
        self.dma_counts[id(owner.dsem)] = (owner.dsem, owner.dcount)
        ev = (owner.dsem, owner.dcount, "dma")
        self._record(ev, reads, writes)
        return ev

    def barrier(self):
        evs = [(E.sem, E.count, "x") for E in self.engs.values() if E.count > 0]
        evs += [(sem, cnt, "dma") for k, (sem, cnt) in self.dma_counts.items() if cnt > 0 and k not in self.no_barrier]
        for E in self.engs.values():
            self._waits(E, (), (), evs)

    def finish(self, bufs):
        self.no_barrier = set()
        self.barrier()


def _constants():
    H = 8
    log_g = np.log1p(-np.exp2(-5.0 - np.arange(H, dtype=np.float64)))
    pos = np.arange(128, dtype=np.float64)
    diff = pos[None, :] - pos[:, None]
    dt_ret = np.where(diff[:, None, :] >= 0, np.exp(np.maximum(diff, 0)[:, None, :] * log_g[None, :, None]), 0.0) / 8.0
    wq_ret = np.exp((pos + 1)[None, None, :] * log_g[None, :, None]) * np.ones((64, 1, 1))
    wk_ret = np.exp((127 - pos)[:, None, None] * log_g[None, :, None]) / 8.0 * np.ones((1, 1, 64))
    g128 = np.exp(128 * log_g)
    slopes = np.exp2(-8.0 * np.arange(1, 13, dtype=np.float64) / 12)
    dil = np.repeat(np.array([1.0, 4.0, 16.0]), 4)
    k = pos[:, None]
    q = pos[None, :]
    eb = np.zeros((128, 12, 2, 128))
    for h in range(12):
        dprev = q - k + 128
        dcur = q - k
        eb[:, h, 0, :] = np.where(k >= q, np.exp(-slopes[h] * dil[h] * dprev), 0.0)
        eb[:, h, 1, :] = np.where(k <= q, np.exp(-slopes[h] * dil[h] * dcur), 0.0)
    return (dt_ret.astype(np.float32), wq_ret.astype(np.float32), wk_ret.astype(np.float32),
            [float(v) for v in g128], eb.astype(np.float32))


FM_RQ, FM_RK, FM_RG, FM_AQ, FM_AK, FM_GR, FM_GA = 0, 512, 1024, 2048, 3584, 5120, 6144
FM_ROWS = 7168
TM_RK, TM_RV, TM_AV = 0, 512, 1536
TM_COLS = 3072


def _slices():
    sl = []
    sl.append(("fm", 0, FM_RQ))
    sl.append(("fm", 512, FM_RK))
    for i in range(2):
        sl.append(("fm", 2048 + 512 * i, FM_RG + 512 * i))
    for i in range(3):
        sl.append(("fm", 3072 + 512 * i, FM_AQ + 512 * i))
    for i in range(3):
        sl.append(("fm", 4608 + 512 * i, FM_AK + 512 * i))
    for i in range(2):
        sl.append(("fm", 7680 + 512 * i, FM_GR + 512 * i))
    for i in range(2):
        sl.append(("fm", 8704 + 512 * i, FM_GA + 512 * i))
    sl.append(("tm", 512, TM_RK))
    for i in range(2):
        sl.append(("tm", 1024 + 512 * i, TM_RV + 512 * i))
    for i in range(3):
        sl.append(("tm", 6144 + 512 * i, TM_AV + 512 * i))
    return sl


def build_nc(S=4096, phases="ABCDEF", debug=False):
    NT = S // 128
    NB5 = S // 512
    dt_ret_np, wq_ret_np, wk_ret_np, G128, eb_np = _constants()
    nc = bass.Bass("TRN2", target_bir_lowering=False)
    dkind = "ExternalOutput" if debug else "Internal"

    def din(name, shape, dt=F32):
        return nc.dram_tensor(name, list(shape), dt, kind="ExternalInput").ap()

    x = din("x", [S, D])
    w_in = din("w_in", [D, IN_W])
    g1b = din("g1b", [128, D])
    g2b = din("g2b", [128, D])
    gfb = din("gfb", [128, D])
    w_ret = din("w_ret", [1024, D])
    w_att = din("w_att", [512, D])
    w_out = din("w_out", [D, D])
    wq = din("wq", [D, 2048])
    skT = din("skT", [128, 16, 128])
    ut = din("ut", [16384, 1024])
    vt = din("vt", [16384, 1024])
    c_dt = din("c_dt", [128, 8, 128])
    c_wq = din("c_wq", [64, 8, 128])
    c_wk = din("c_wk", [128, 8, 64])
    c_eb = din("c_eb", [128, 12, 2, 128])
    out = nc.dram_tensor("out", [S, D], F32, kind="ExternalOutput").ap()

    fm = nc.dram_tensor("fm", [FM_ROWS, S], BF16, kind=dkind).ap()
    tm = nc.dram_tensor("tm", [S, TM_COLS], BF16, kind=dkind).ap()
    retT = nc.dram_tensor("retT", [1024, S], BF16, kind=dkind).ap()
    og = nc.dram_tensor("og", [3, S, 4, 132], F32, kind=dkind).ap()
    hscr = nc.dram_tensor("hscr", [S, D], F32, kind=dkind).ap()
    xn2T = nc.dram_tensor("xn2T", [D, S], BF16, kind=dkind).ap()
    iaT = nc.dram_tensor("iaT", [128, S], BF16, kind=dkind).ap()
    ibT = nc.dram_tensor("ibT", [128, S], BF16, kind=dkind).ap()
    GT = nc.dram_tensor("GT", [128, S], BF16, kind=dkind).ap()
    ubf = nc.dram_tensor("ubf", [16384, 1024], BF16, kind="Internal").ap()
    vbf = nc.dram_tensor("vbf", [16384, 1024], BF16, kind="Internal").ap()

    with ExitStack() as top:
        top.enter_context(nc.allow_low_precision("one-hot index decode sums are exact small integers; gates are bf16 matmul operands"))
        fw = FW(nc, top)

        def sbt(st, name, shape, dt):
            return st.enter_context(nc.sbuf_tensor(name, list(shape), dt))

        ps = [top.enter_context(nc.psum_tensor(f"ps{i}", [128, 512], F32)) for i in range(8)]
        b_ps = fw.bufs(8, "ps")
        b_fm = fw.buf("fm", multi=True)
        b_tm = fw.buf("tm", multi=True)
        b_retT = fw.buf("retT", multi=True)
        b_og = fw.buf("og", multi=True)
        b_h = fw.buf("hscr", multi=True)
        b_xn2T = fw.buf("xn2T", multi=True)
        b_sel = fw.buf("sel", multi=True)
        b_ubf = fw.buf("ubf", multi=True)
        b_vbf = fw.buf("vbf", multi=True)
        b_conv = fw.bufs(16, "conv", dma=True, sw=True)
        b_out = fw.buf("out", multi=True)
        fw.no_barrier = {id(b.dsem) for b in b_conv}

        def conv_piece(i):
            src, dst, tok = (ut, ubf, b_ubf) if i < 8 else (vt, vbf, b_vbf)
            r0 = (i % 8) * 2048
            fw.dma("pool", b_conv[i], [(lambda h, j=j: h.dma_start(out=dst[r0 + j * 1024:r0 + (j + 1) * 1024, :],
                                                                 in_=src[r0 + j * 1024:r0 + (j + 1) * 1024, :])) for j in range(2)],
                   writes=[tok])

        ident = sbt(top, "ident", [128, 128], BF16)
        identf = sbt(top, "identf", [128, 128], F32)
        iota_f = sbt(top, "iota_f", [128, 128], F32)
        b_const = fw.buf("const")
        fw.op("pool", lambda h: h.iota(identf[:], pattern=[[1, 128]], base=0, channel_multiplier=-1,
                                       allow_small_or_imprecise_dtypes=True), writes=[b_const])
        fw.op("dve", lambda h: h.tensor_single_scalar(out=ident[:], in_=identf[:], scalar=0.0, op=ALU.is_equal),
              reads=[b_const], writes=[b_const])
        fw.op("dve", lambda h: h.tensor_single_scalar(out=identf[:], in_=identf[:], scalar=0.0, op=ALU.is_equal),
              reads=[b_const], writes=[b_const])
        fw.op("pool", lambda h: h.iota(iota_f[:], pattern=[[1, 128]], base=0, channel_multiplier=0,
                                       allow_small_or_imprecise_dtypes=True), writes=[b_const])

        evac_ctr = [0]

        def evac(out_ap, in_ap, reads, writes):
            evac_ctr[0] += 1
            if evac_ctr[0] % 2:
                fw.op("act", lambda h: h.activation(out=out_ap, in_=in_ap, func=AF.Copy), reads=reads, writes=writes)
            else:
                fw.op("dve", lambda h: h.tensor_copy(out=out_ap, in_=in_ap), reads=reads, writes=writes)

        def rms_rstd(st_tile, ss_ap, rstd_ap, b_ss, src_ap, junk_ap, b_src, b_junk, n):
            fw.op("act", lambda h: h.activation(out=junk_ap, in_=src_ap, func=AF.Square, accum_out=ss_ap),
                  reads=[b_src], writes=[b_junk, b_ss])
            fw.op("act", lambda h: h.activation(out=rstd_ap, in_=ss_ap, func=AF.Sqrt, bias=EPS, scale=1.0 / n),
                  reads=[b_ss], writes=[b_ss])
            fw.op("dve", lambda h: h.reciprocal(out=rstd_ap, in_=rstd_ap), reads=[b_ss], writes=[b_ss])

        if "A" in phases:
            with ExitStack() as st:
                xT = sbt(st, "xT", [128, 8, S], BF16)
                b_xT = fw.bufs(NT, "xT")
                xin = [sbt(st, f"xin{i}", [128, D], F32) for i in range(3)]
                b_xin = fw.bufs(3, "xin", dma=True, stack=st)
                xb = [sbt(st, f"xb{i}", [128, D], BF16) for i in range(2)]
                b_xb = fw.bufs(2, "xb")
                junk = sbt(st, "junkA", [128, D], BF16)
                b_junk = fw.buf("junkA")
                g1 = sbt(st, "g1", [128, D], F32)
                b_g1 = fw.buf("g1", dma=True, stack=st)
                ssA = sbt(st, "ssA", [128, NT], F32)
                rsA = sbt(st, "rsA", [128, NT], F32)
                b_ssA = fw.bufs(NT, "ssA")
                wbf = [sbt(st, f"wbf{i}", [128, 8, 512], BF16) for i in range(2)]
                b_wbf = fw.bufs(2, "wbf", dma=True, stack=st, sw=True)
                ost = [sbt(st, f"ostA{i}", [128, 4, 512], BF16) for i in range(2)]
                b_ost = fw.bufs(2, "ostA", dma=True, stack=st)

                fw.dma("sp", b_g1, [lambda h: h.dma_start(out=g1[:], in_=g1b)], writes=[b_g1])
                slices = _slices()

                def load_w(si):
                    kind, c0, d0 = slices[si]
                    sl = si % 2
                    src = w_in[:, c0:c0 + 512].rearrange("(c p) n -> p c n", p=128)
                    fw.dma("pool", b_wbf[sl], [lambda h: h.dma_start(out=wbf[sl][:], in_=src)], writes=[b_wbf[sl]])

                load_w(0)
                load_w(1)
                def prep_tile(t):
                    s3 = t % 3
                    s2 = t % 2
                    fw.dma("sp", b_xin[s3], [lambda h: h.dma_start(out=xin[s3][:], in_=x[t * 128:(t + 1) * 128, :])],
                           writes=[b_xin[s3]])
                    rms_rstd(None, ssA[:, t:t + 1], rsA[:, t:t + 1], b_ssA[t], xin[s3][:], junk[:], b_xin[s3], b_junk, D)
                    fw.op("dve", lambda h: h.scalar_tensor_tensor(out=xb[s2][:], in0=xin[s3][:], scalar=rsA[:, t:t + 1],
                                                                  in1=g1[:], op0=ALU.mult, op1=ALU.mult),
                          reads=[b_xin[s3], b_ssA[t], b_g1], writes=[b_xb[s2]])
                    pb = s2
                    psb = ps[pb][:].bitcast(BF16)
                    for c in range(8):
                        fw.op("pe", lambda h: h.transpose(out=psb[:, c * 128:(c + 1) * 128], in_=xb[s2][:, c * 128:(c + 1) * 128],
                                                          identity=ident[:]),
                              reads=[b_xb[s2], b_const], writes=[b_ps[pb]])
                    evac(xT[:, :, t * 128:(t + 1) * 128], psb.rearrange("p (c j) -> p c j", c=8), [b_ps[pb]], [b_xT[t]])

                for t in range(8):
                    prep_tile(t)
                pctr = 0
                octr = 0
                for si, (kind, c0, d0) in enumerate(slices):
                    sl = si % 2
                    if kind == "fm":
                        for tb in range(NB5):
                            if si == 0:
                                for t in range(4 * (tb + 2), min(NT, 4 * (tb + 3))):
                                    prep_tile(t)
                            os_ = octr % 2
                            octr += 1
                            for q in range(4):
                                pb = 2 + pctr % 6
                                pctr += 1
                                for c in range(8):
                                    fw.op("pe", lambda h: h.matmul(ps[pb][:], lhsT=wbf[sl][:, c, q * 128:(q + 1) * 128],
                                                                   rhs=xT[:, c, tb * 512:(tb + 1) * 512], start=(c == 0), stop=(c == 7)),
                                          reads=[b_wbf[sl]] + b_xT[tb * 4:tb * 4 + 4], writes=[b_ps[pb]], inc=(c == 7))
                                evac(ost[os_][:, q, :], ps[pb][:], [b_ps[pb]], [b_ost[os_]])
                            dst = fm[d0:d0 + 512, tb * 512:(tb + 1) * 512].rearrange("(q p) j -> p q j", p=128)
                            fw.dma("sp", b_ost[os_], [lambda h: h.dma_start(out=dst, in_=ost[os_][:])],
                                   reads=[b_ost[os_]], writes=[b_fm])
                    else:
                        for t4 in range(NT // 4):
                            os_ = octr % 2
                            octr += 1
                            for u in range(4):
                                t = t4 * 4 + u
                                pb = 2 + pctr % 6
                                pctr += 1
                                for c in range(8):
                                    fw.op("pe", lambda h: h.matmul(ps[pb][:], lhsT=xT[:, c, t * 128:(t + 1) * 128],
                                                                   rhs=wbf[sl][:, c, :], start=(c == 0), stop=(c == 7)),
                                          reads=[b_wbf[sl], b_xT[t]], writes=[b_ps[pb]], inc=(c == 7))
                                evac(ost[os_][:, u, :], ps[pb][:], [b_ps[pb]], [b_ost[os_]])
                            dst = tm[t4 * 512:(t4 + 1) * 512, d0:d0 + 512].rearrange("(u p) n -> p u n", p=128)
                            fw.dma("sp", b_ost[os_], [lambda h: h.dma_start(out=dst, in_=ost[os_][:])],
                                   reads=[b_ost[os_]], writes=[b_tm])
                    if si + 2 < len(slices):
                        load_w(si + 2)

        if "B" in phases:
            fw.barrier()
            with ExitStack() as st:
                qTb = [sbt(st, f"qTb{i}", [64, 8, 512], BF16) for i in range(2)]
                kTb = [sbt(st, f"kTb{i}", [64, 8, 512], BF16) for i in range(2)]
                gTb = [sbt(st, f"gTb{i}", [128, 8, 512], BF16) for i in range(2)]
                ktm = [sbt(st, f"ktm{i}", [128, 4, 512], BF16) for i in range(2)]
                vtm = [sbt(st, f"vtm{i}", [128, 4, 1024], BF16) for i in range(2)]
                b_ld = fw.bufs(2, "ldB", dma=True, stack=st)
                cdt = sbt(st, "cdt", [128, 8, 128], F32)
                cwq = sbt(st, "cwq", [64, 8, 128], F32)
                cwk = sbt(st, "cwk", [128, 8, 64], F32)
                b_cB = fw.buf("cB", dma=True, stack=st)
                ones = sbt(st, "ones", [128, 128], BF16)
                qs = [sbt(st, f"qs{i}", [64, 8, 128], BF16) for i in range(2)]
                b_qs = fw.bufs(2, "qs")
                kw = [sbt(st, f"kw{i}", [128, 512], BF16) for i in range(2)]
                b_kw = fw.bufs(2, "kw")
                A = [sbt(st, f"A{i}", [128, 4, 128], BF16) for i in range(2)]
                b_A = fw.bufs(2, "A")
                sq = [sbt(st, f"sq{i}", [128, 512], BF16) for i in range(2)]
                b_sq = fw.bufs(2, "sq")
                rs = [sbt(st, f"rs{i}", [128, 512], F32) for i in range(2)]
                b_rs = fw.bufs(2, "rs")
                sg = [sbt(st, f"sg{i}", [128, 4, 128], BF16) for i in range(2)]
                b_sg = fw.bufs(2, "sg")
                tt = [sbt(st, f"tt{i}", [128, 512], BF16) for i in range(2)]
                b_tt = fw.bufs(2, "tt")
                yb = [sbt(st, f"yb{i}", [128, 8, 512], BF16) for i in range(2)]
                b_yb = fw.bufs(2, "yb", dma=True, stack=st)
                R32 = sbt(st, "R32", [64, 8, 128], F32)
                b_R32 = fw.bufs(2, "R32")
                Rb = [sbt(st, f"Rb{i}", [64, 8, 128], BF16) for i in range(2)]
                b_Rb = [fw.bufs(2, f"Rb{i}_") for i in range(2)]

                fw.dma("sp", b_cB, [lambda h: h.dma_start(out=cdt[:], in_=c_dt),
                                    lambda h: h.dma_start(out=cwq[:], in_=c_wq),
                                    lambda h: h.dma_start(out=cwk[:], in_=c_wk)], writes=[b_cB])
                fw.op("dve", lambda h: h.memset(ones[:], 1.0 / 128), writes=[b_const])
                fw.op("dve", lambda h: h.memset(R32[:], 0.0), writes=b_R32)

                def load_blk(tb):
                    sl = tb % 2
                    cs = slice(tb * 512, (tb + 1) * 512)
                    rs_ = slice(tb * 512, (tb + 1) * 512)
                    fw.dma("sp", b_ld[sl], [
                        lambda h: h.dma_start(out=qTb[sl][:], in_=fm[FM_RQ:FM_RQ + 512, cs].rearrange("(h d) j -> d h j", d=64)),
                        lambda h: h.dma_start(out=kTb[sl][:], in_=fm[FM_RK:FM_RK + 512, cs].rearrange("(h d) j -> d h j", d=64)),
                        lambda h: h.dma_start(out=gTb[sl][:], in_=fm[FM_RG:FM_RG + 1024, cs].rearrange("(h e) j -> e h j", e=128)),
                        lambda h: h.dma_start(out=ktm[sl][:], in_=tm[rs_, TM_RK:TM_RK + 512].rearrange("(u p) n -> p u n", p=128)),
                        lambda h: h.dma_start(out=vtm[sl][:], in_=tm[rs_, TM_RV:TM_RV + 1024].rearrange("(u p) n -> p u n", p=128)),
                    ], reads=[b_fm, b_tm], writes=[b_ld[sl]])

                load_blk(0)

                def ctx(i):
                    n, half = divmod(i, 2)
                    tb, u = divmod(n, 4)
                    return n, half, tb, u, tb % 2, slice(u * 128, (u + 1) * 128), n % 2, i % 2

                def chunk_setup(n):
                    tb, u = divmod(n, 4)
                    sl = tb % 2
                    cs = slice(u * 128, (u + 1) * 128)
                    s = n % 2
                    fw.op("pool", lambda h: h.tensor_tensor(out=qs[s][:], in0=qTb[sl][:, :, cs], in1=cwq[:], op=ALU.mult),
                          reads=[b_ld[sl], b_cB], writes=[b_qs[s]])
                    fw.op("pool", lambda h: h.tensor_tensor(out=kw[s][:], in0=ktm[sl][:, u, :],
                                                            in1=cwk[:].rearrange("p h d -> p (h d)"), op=ALU.mult),
                          reads=[b_ld[sl], b_cB], writes=[b_kw[s]])

                def stage1(i):
                    n, half, tb, u, sl, cs, s, s2 = ctx(i)
                    p_st = s2
                    hs = slice(4 * half, 4 * half + 4)
                    for hh in range(4):
                        hd = 4 * half + hh
                        fw.op("pe", lambda h: h.matmul(ps[p_st][:, hh * 128:(hh + 1) * 128], lhsT=kTb[sl][:, hd, cs],
                                                       rhs=qTb[sl][:, hd, cs], start=True, stop=True),
                              reads=[b_ld[sl]], writes=[b_ps[p_st]])
                    fw.op("dve", lambda h: h.tensor_tensor(out=A[s2][:], in0=ps[p_st][:].rearrange("p (h i) -> p h i", h=4),
                                                           in1=cdt[:, hs, :], op=ALU.mult),
                          reads=[b_ps[p_st], b_cB], writes=[b_A[s2]])

                def stage2(i):
                    n, half, tb, u, sl, cs, s, s2 = ctx(i)
                    rsl = n % 2
                    p_o, p_kv = 2 + i % 3, 5
                    hs = slice(4 * half, 4 * half + 4)
                    for hh in range(4):
                        hd = 4 * half + hh
                        fw.op("pe", lambda h: h.matmul(ps[p_o][:, hh * 128:(hh + 1) * 128],
                                                       lhsT=vtm[sl][:, u, hd * 128:(hd + 1) * 128], rhs=A[s2][:, hh, :],
                                                       start=True, stop=(n == 0)),
                              reads=[b_ld[sl], b_A[s2]], writes=[b_ps[p_o]])
                        if n > 0:
                            fw.op("pe", lambda h: h.matmul(ps[p_o][:, hh * 128:(hh + 1) * 128],
                                                           lhsT=Rb[rsl][:, hd, :], rhs=qs[s][:, hd, :], start=False, stop=True),
                                  reads=[b_Rb[rsl][half], b_qs[s]], writes=[b_ps[p_o]])
                    if n + 1 < NT:
                        for hh in range(4):
                            hd = 4 * half + hh
                            fw.op("pe", lambda h: h.matmul(ps[p_kv][0:64, hh * 128:(hh + 1) * 128],
                                                           lhsT=kw[s][:, hd * 64:(hd + 1) * 64],
                                                           rhs=vtm[sl][:, u, hd * 128:(hd + 1) * 128], start=True, stop=True),
                                  reads=[b_kw[s], b_ld[sl]], writes=[b_ps[p_kv]])
                    fw.op("act", lambda h: h.activation(out=sq[s2][:], in_=ps[p_o][:], func=AF.Square),
                          reads=[b_ps[p_o]], writes=[b_sq[s2]])
                    fw.op("act", lambda h: h.activation(out=sg[s2][:], in_=gTb[sl][:, hs, cs], func=AF.Silu),
                          reads=[b_ld[sl]], writes=[b_sg[s2]])
                    if n + 1 < NT:
                        for hh in range(4):
                            hd = 4 * half + hh
                            fw.op("dve", lambda h: h.scalar_tensor_tensor(out=R32[:, hd, :], in0=R32[:, hd, :], scalar=G128[hd],
                                                                          in1=ps[p_kv][0:64, hh * 128:(hh + 1) * 128],
                                                                          op0=ALU.mult, op1=ALU.add),
                                  reads=[b_ps[p_kv], b_R32[half]], writes=[b_R32[half]])
                        fw.op("act", lambda h: h.activation(out=Rb[1 - rsl][:, hs, :], in_=R32[:, hs, :], func=AF.Copy),
                              reads=[b_R32[half]], writes=[b_Rb[1 - rsl][half]])

                def stage3(i):
                    n, half, tb, u, sl, cs, s, s2 = ctx(i)
                    p_o, p_ss = 2 + i % 3, 6 + s2
                    hs = slice(4 * half, 4 * half + 4)
                    ys = tb % 2
                    fw.op("pe", lambda h: h.matmul(ps[p_ss][:], lhsT=ones[:], rhs=sq[s2][:], start=True, stop=True),
                          reads=[b_sq[s2], b_const], writes=[b_ps[p_ss]])
                    fw.op("act", lambda h: h.activation(out=rs[s2][:], in_=ps[p_ss][:], func=AF.Sqrt, bias=EPS, scale=1.0),
                          reads=[b_ps[p_ss]], writes=[b_rs[s2]])
                    fw.op("dve", lambda h: h.reciprocal(out=rs[s2][:], in_=rs[s2][:]), reads=[b_rs[s2]], writes=[b_rs[s2]])
                    fw.op("dve", lambda h: h.tensor_tensor(out=tt[s2][:], in0=ps[p_o][:], in1=rs[s2][:], op=ALU.mult),
                          reads=[b_ps[p_o], b_rs[s2]], writes=[b_tt[s2]])
                    fw.op("pool", lambda h: h.tensor_tensor(out=yb[ys][:, hs, cs], in0=tt[s2][:].rearrange("p (h i) -> p h i", h=4),
                                                            in1=sg[s2][:], op=ALU.mult),
                          reads=[b_tt[s2], b_sg[s2]], writes=[b_yb[ys]])
                    if u == 3 and half == 1:
                        dst = retT[:, tb * 512:(tb + 1) * 512].rearrange("(h e) j -> e h j", e=128)
                        fw.dma("sp", b_yb[ys], [lambda h: h.dma_start(out=dst, in_=yb[ys][:])], reads=[b_yb[ys]], writes=[b_retT])

                NI = 2 * NT
                chunk_setup(0)
                stage1(0)
                for i in range(NI + 1):
                    if i + 1 < NI:
                        if (i + 1) % 2 == 0:
                            chunk_setup((i + 1) // 2)
                        stage1(i + 1)
                    if i < NI:
                        stage2(i)
                    if i - 1 >= 0:
                        stage3(i - 1)
                    if i % 8 == 0 and i // 8 + 1 < NB5:
                        load_blk(i // 8 + 1)

        if "C" in phases:
            fw.barrier()
            with ExitStack() as st0:
                ebf = sbt(st0, "ebf", [128, 12, 2, 128], F32)
                eb = sbt(st0, "eb", [128, 12, 2, 128], BF16)
                b_eb = fw.buf("eb", dma=True, stack=st0)
                fw.dma("sp", b_eb, [lambda h: h.dma_start(out=ebf[:], in_=c_eb)], writes=[b_eb])
                fw.op("dve", lambda h: h.tensor_copy(out=eb[:], in_=ebf[:]), reads=[b_eb], writes=[b_eb])
                E = [sbt(st0, f"E{i}", [128, 4, 256], BF16) for i in range(2)]
                b_E = fw.bufs(2, "E")
                P = [sbt(st0, f"P{i}", [128, 4, 256], BF16) for i in range(2)]
                b_P = fw.bufs(2, "P")
                osb = [sbt(st0, f"osb{i}", [128, 4, 132], F32) for i in range(2)]
                b_osb = fw.bufs(2, "osb", dma=True, stack=st0, sw=True)
                b_osbj = [fw.bufs(2, f"osbj{i}_") for i in range(2)]
                for i in range(2):
                    fw.op("dve", lambda h: h.memset(osb[i][:], 0.0), writes=[b_osb[i]] + b_osbj[i])
                scale = 128.0 ** -0.5
                it = 0
                for g, dil in enumerate((1, 4, 16)):
                    SB = 128 * dil
                    nsb = S // SB
                    fw.barrier()
                    with ExitStack() as st:
                        NS = min(3, nsb)
                        qTs = [sbt(st, f"qTs{g}_{i}", [128, 4, SB], BF16) for i in range(2)]
                        b_qTs = fw.bufs(2, "qTs", dma=True, stack=st)
                        kTs = [sbt(st, f"kTs{g}_{i}", [128, 4, SB], BF16) for i in range(NS)]
                        Vs = [sbt(st, f"Vs{g}_{i}", [128, dil, 4, 130], BF16) for i in range(NS)]
                        b_kv = fw.bufs(NS, "kvs", dma=True, stack=st)
                        for i in range(NS):
                            fw.op("dve", lambda h: h.memset(Vs[i][:, :, :, 128:130], 1.0), writes=[b_kv[i]])

                        wide = (dil == 1)
                        if wide:
                            NTW = S // 512
                            qw = [sbt(st, f"qw{i}", [128, 4, 512], BF16) for i in range(2)]
                            kwd = [sbt(st, f"kwd{i}", [128, 4, 512], BF16) for i in range(3)]
                            b_qw = fw.bufs(2, "qw", dma=True, stack=st)
                            b_kwd = fw.bufs(3, "kwd", dma=True, stack=st)

                            def load_wide(T):
                                csw = slice(T * 512, (T + 1) * 512)
                                fw.dma("sp", b_qw[T % 2], [lambda h: h.dma_start(
                                    out=qw[T % 2][:], in_=fm[FM_AQ + g * 512:FM_AQ + (g + 1) * 512, csw].rearrange("(h d) j -> d h j", d=128))],
                                    reads=[b_fm], writes=[b_qw[T % 2]])
                                fw.dma("sp", b_kwd[T % 3], [lambda h: h.dma_start(
                                    out=kwd[T % 3][:], in_=fm[FM_AK + g * 512:FM_AK + (g + 1) * 512, csw].rearrange("(h d) j -> d h j", d=128))],
                                    reads=[b_fm], writes=[b_kwd[T % 3]])

                        def q_op(n, hh, r):
                            if wide:
                                T, j = divmod(n, 4)
                                return qw[T % 2][:, hh, j * 128:(j + 1) * 128], b_qw[T % 2]
                            return qTs[n % 2][:, hh, r::dil], b_qTs[n % 2]

                        def k_op(n, hh, r):
                            if wide:
                                T, j = divmod(n, 4)
                                return kwd[T % 3][:, hh, j * 128:(j + 1) * 128], b_kwd[T % 3]
                            return kTs[n % NS][:, hh, r::dil], b_kv[n % NS]

                        def load_sb(n):
                            cs = slice(n * SB, (n + 1) * SB)
                            s2 = n % 2
                            s3 = n % NS
                            vsrc = tm[cs, TM_AV + g * 512:TM_AV + (g + 1) * 512].rearrange("(i r) (h e) -> i r h e", r=dil, h=4)
                            fns = []
                            if not wide:
                                fw.dma("sp", b_qTs[s2], [lambda h: h.dma_start(
                                    out=qTs[s2][:], in_=fm[FM_AQ + g * 512:FM_AQ + (g + 1) * 512, cs].rearrange("(h d) j -> d h j", d=128))],
                                    reads=[b_fm], writes=[b_qTs[s2]])
                                fns.append(lambda h: h.dma_start(
                                    out=kTs[s3][:], in_=fm[FM_AK + g * 512:FM_AK + (g + 1) * 512, cs].rearrange("(h d) j -> d h j", d=128)))
                            for r in range(dil):
                                fns.append(lambda h, r=r: h.dma_start(out=Vs[s3][:, r, :, 0:128], in_=vsrc[:, r]))
                            fw.dma("sp", b_kv[s3], fns, reads=[b_fm, b_tm], writes=[b_kv[s3]])

                        if wide:
                            load_wide(0)
                        load_sb(0)
                        for n in range(nsb):
                            if wide and n % 4 == 0 and n // 4 + 1 < NTW:
                                load_wide(n // 4 + 1)
                            if n + 1 < nsb:
                                load_sb(n + 1)
                            s2 = n % 2
                            cur = n % NS
                            prv = (n - 1) % NS
                            kprev = prv if n > 0 else cur
                            nprev = n - 1 if n > 0 else n
                            for r in range(dil):
                                w = it % 2
                                it += 1
                                p_s = [w * 2, w * 2 + 1]
                                p_o = [4 + w * 2, 4 + w * 2 + 1]
                                for hh in range(4):
                                    pb = p_s[hh // 2]
                                    o0 = (hh % 2) * 256
                                    q_ap, q_tok = q_op(n, hh, r)
                                    kp_ap, kp_tok = k_op(nprev, hh, r)
                                    kc_ap, kc_tok = k_op(n, hh, r)
                                    fw.op("pe", lambda h: h.matmul(ps[pb][:, o0:o0 + 128], lhsT=kp_ap, rhs=q_ap, start=True, stop=True),
                                          reads=[kp_tok, q_tok], writes=[b_ps[pb]])
                                    fw.op("pe", lambda h: h.matmul(ps[pb][:, o0 + 128:o0 + 256], lhsT=kc_ap, rhs=q_ap, start=True, stop=True),
                                          reads=[kc_tok, q_tok], writes=[b_ps[pb]])
                                for j in range(2):
                                    fw.op("act", lambda h: h.activation(out=E[w][:, 2 * j:2 * j + 2, :].rearrange("p a b -> p (a b)"),
                                                                        in_=ps[p_s[j]][:], func=AF.Exp, scale=scale),
                                          reads=[b_ps[p_s[j]]], writes=[b_E[w]])
                                fw.op("dve", lambda h: h.tensor_tensor(out=P[w][:], in0=E[w][:],
                                                                       in1=eb[:, 4 * g:4 * g + 4, :, :].rearrange("p h t q -> p h (t q)"),
                                                                       op=ALU.mult),
                                      reads=[b_E[w], b_eb], writes=[b_P[w]])
                                for hh in range(4):
                                    pb = p_o[hh // 2]
                                    o0 = (hh % 2) * 256
                                    if n > 0:
                                        fw.op("pe", lambda h: h.matmul(ps[pb][:, o0:o0 + 129], lhsT=P[w][:, hh, 0:128],
                                                                       rhs=Vs[prv][:, r, hh, 0:129], start=True, stop=False),
                                              reads=[b_P[w], b_kv[prv]], writes=[b_ps[pb]])
                                    fw.op("pe", lambda h: h.matmul(ps[pb][:, o0:o0 + 129], lhsT=P[w][:, hh, 128:256],
                                                                   rhs=Vs[cur][:, r, hh, 0:129], start=(n == 0), stop=True),
                                          reads=[b_P[w], b_kv[cur]], writes=[b_ps[pb]])
                                for j in range(2):
                                    evac(osb[w][:, 2 * j:2 * j + 2, 0:129],
                                         ps[p_o[j]][:].rearrange("p (a b) -> p a b", a=2)[:, :, 0:129],
                                         [b_ps[p_o[j]], b_osb[w]], [b_osbj[w][j]])
                                dst = og[g, n * SB:(n + 1) * SB].rearrange("(i r) h e -> i r h e", r=dil)[:, r]
                                fw.dma("pool", b_osb[w], [lambda h: h.dma_start(out=dst, in_=osb[w][:])], reads=b_osbj[w], writes=[b_og, b_osb[w]])

        if "D" in phases:
            fw.barrier()
            with ExitStack() as st:
                Wret = sbt(st, "Wret", [128, 8, D], BF16)
                Watt = sbt(st, "Watt", [128, 4, D], BF16)
                Wout = sbt(st, "Wout", [128, 8, D], BF16)
                g2 = sbt(st, "g2", [128, D], F32)
                b_W = fw.buf("WD", dma=True, stack=st, sw=True)
                fw.dma("pool", b_W, [
                    lambda h: h.dma_start(out=Wret[:], in_=w_ret.rearrange("(c p) n -> p c n", p=128)),
                    lambda h: h.dma_start(out=Watt[:], in_=w_att.rearrange("(c p) n -> p c n", p=128)),
                    lambda h: h.dma_start(out=Wout[:], in_=w_out.rearrange("(c p) n -> p c n", p=128)),
                ], writes=[b_W])
                b_g2 = fw.buf("g2", dma=True, stack=st)
                fw.dma("sp", b_g2, [lambda h: h.dma_start(out=g2[:], in_=g2b)], writes=[b_g2])
                rTb = [sbt(st, f"rTb{i}", [128, 8, 512], BF16) for i in range(2)]
                grb = [sbt(st, f"grb{i}", [128, 8, 512], BF16) for i in range(2)]
                gab = [sbt(st, f"gab{i}", [128, 8, 512], BF16) for i in range(2)]
                b_ldD = fw.bufs(2, "ldD", dma=True, stack=st)
                ogt = [sbt(st, f"ogt{i}", [128, 3, 4, 132], F32) for i in range(2)]
                b_ogt = fw.bufs(2, "ogt", dma=True, stack=st)
                xt = [sbt(st, f"xtD{i}", [128, D], F32) for i in range(2)]
                b_xt = fw.bufs(2, "xtD", dma=True, stack=st)
                acc = sbt(st, "acc", [128, 4, 132], F32)
                b_acc = fw.buf("acc")
                rden = sbt(st, "rden", [128, 4, 1], F32)
                attn = sbt(st, "attn", [128, 4, 128], BF16)
                b_attn = fw.buf("attn")
                aTb = sbt(st, "aTb", [128, 4, 512], BF16)
                b_aTb = fw.bufs(4, "aTb")
                sgr = [sbt(st, f"sgr{i}", [128, 512], BF16) for i in range(2)]
                sga = [sbt(st, f"sga{i}", [128, 512], BF16) for i in range(2)]
                b_sgg = fw.bufs(2, "sgg")
                m1 = [sbt(st, f"m1{i}", [128, 512], F32) for i in range(2)]
                m2 = [sbt(st, f"m2{i}", [128, 512], F32) for i in range(2)]
                b_m = fw.bufs(2, "m")
                mT = sbt(st, "mT", [128, 8, 512], BF16)
                b_mT = fw.bufs(8, "mT")
                hh_ = [sbt(st, f"hD{i}", [128, D], F32) for i in range(2)]
                b_hh = fw.bufs(2, "hD", dma=True, stack=st)
                junk = sbt(st, "junkD", [128, D], BF16)
                b_junk = fw.buf("junkD")
                ssD = sbt(st, "ssD", [128, NT], F32)
                rsD = sbt(st, "rsD", [128, NT], F32)
                b_ssD = fw.bufs(NT, "ssD")
                xn = [sbt(st, f"xnD{i}", [128, D], BF16) for i in range(2)]
                b_xn = fw.bufs(2, "xnD")
                xnTb = [sbt(st, f"xnTb{i}", [128, 8, 512], BF16) for i in range(2)]
                b_xnTb = fw.bufs(2, "xnTb", dma=True, stack=st)

                def load_blkD(tb):
                    sl = tb % 2
                    cs = slice(tb * 512, (tb + 1) * 512)
                    fw.dma("sp", b_ldD[sl], [
                        lambda h: h.dma_start(out=rTb[sl][:], in_=retT[:, cs].rearrange("(c p) j -> p c j", p=128)),
                        lambda h: h.dma_start(out=grb[sl][:], in_=fm[FM_GR:FM_GR + 1024, cs].rearrange("(c p) j -> p c j", p=128)),
                        lambda h: h.dma_start(out=gab[sl][:], in_=fm[FM_GA:FM_GA + 1024, cs].rearrange("(c p) j -> p c j", p=128)),
                    ], reads=[b_fm, b_retT], writes=[b_ldD[sl]])

                def load_og(t):
                    s2 = t % 2
                    rows = slice(t * 128, (t + 1) * 128)
                    fw.dma("sp", b_ogt[s2], [(lambda h, g=g: h.dma_start(out=ogt[s2][:, g], in_=og[g, rows])) for g in range(3)],
                           reads=[b_og], writes=[b_ogt[s2]])

                def load_x(t):
                    s2 = t % 2
                    rows = slice(t * 128, (t + 1) * 128)
                    fw.dma("sp", b_xt[s2], [lambda h: h.dma_start(out=xt[s2][:], in_=x[rows, :])], writes=[b_xt[s2]])

                load_blkD(0)
                load_og(0)
                load_x(0)
                pc = 0
                pcD = [1000]
                for tb in range(NB5):
                    sl = tb % 2
                    if tb + 1 < NB5:
                        load_blkD(tb + 1)
                    for u in range(4):
                        t = tb * 4 + u
                        s2 = t % 2
                        if t + 1 < NT:
                            load_og(t + 1)
                        fw.op("dve", lambda h: h.tensor_tensor(out=acc[:], in0=ogt[s2][:, 0], in1=ogt[s2][:, 1], op=ALU.add),
                              reads=[b_ogt[s2]], writes=[b_acc])
                        fw.op("dve", lambda h: h.tensor_tensor(out=acc[:], in0=acc[:], in1=ogt[s2][:, 2], op=ALU.add),
                              reads=[b_ogt[s2], b_acc], writes=[b_acc])
                        fw.op("dve", lambda h: h.reciprocal(out=rden[:], in_=acc[:, :, 128:129]), reads=[b_acc], writes=[b_acc])
                        fw.op("dve", lambda h: h.tensor_tensor(out=attn[:], in0=acc[:, :, 0:128], in1=rden[:].to_broadcast([128, 4, 128]),
                                                               op=ALU.mult), reads=[b_acc], writes=[b_attn])
                        pb = pc % 2
                        pc += 1
                        psb = ps[pb][:].bitcast(BF16)
                        for c in range(4):
                            fw.op("pe", lambda h: h.transpose(out=psb[:, c * 128:(c + 1) * 128], in_=attn[:, c, :], identity=ident[:]),
                                  reads=[b_attn, b_const], writes=[b_ps[pb]])
                        evac(aTb[:, :, u * 128:(u + 1) * 128], psb[:, 0:512].rearrange("p (c j) -> p c j", c=4), [b_ps[pb]], [b_aTb[u]])
                        if t + 1 < NT:
                            pass
                    for ncn in range(8):
                        w2 = ncn % 2
                        p_r, p_a = 2 + w2, 4 + w2
                        for c in range(8):
                            fw.op("pe", lambda h: h.matmul(ps[p_r][:], lhsT=Wret[:, c, ncn * 128:(ncn + 1) * 128], rhs=rTb[sl][:, c, :],
                                                           start=(c == 0), stop=(c == 7)),
                                  reads=[b_W, b_ldD[sl]], writes=[b_ps[p_r]], inc=(c == 7))
                        for c in range(4):
                            fw.op("pe", lambda h: h.matmul(ps[p_a][:], lhsT=Watt[:, c, ncn * 128:(ncn + 1) * 128], rhs=aTb[:, c, :],
                                                           start=(c == 0), stop=(c == 3)),
                                  reads=[b_W] + b_aTb, writes=[b_ps[p_a]], inc=(c == 3))
                        fw.op("act", lambda h: h.activation(out=sgr[w2][:], in_=grb[sl][:, ncn, :], func=AF.Sigmoid),
                              reads=[b_ldD[sl]], writes=[b_sgg[w2]])
                        fw.op("act", lambda h: h.activation(out=sga[w2][:], in_=gab[sl][:, ncn, :], func=AF.Sigmoid),
                              reads=[b_ldD[sl]], writes=[b_sgg[w2]])
                        fw.op("dve", lambda h: h.tensor_tensor(out=m1[w2][:], in0=ps[p_r][:], in1=sgr[w2][:], op=ALU.mult),
                              reads=[b_ps[p_r], b_sgg[w2]], writes=[b_m[w2]])
                        fw.op("dve", lambda h: h.tensor_tensor(out=m2[w2][:], in0=ps[p_a][:], in1=sga[w2][:], op=ALU.mult),
                              reads=[b_ps[p_a], b_sgg[w2]], writes=[b_m[w2]])
                        fw.op("dve", lambda h: h.tensor_tensor(out=mT[:, ncn, :], in0=m1[w2][:], in1=m2[w2][:], op=ALU.add),
                              reads=[b_m[w2]], writes=[b_mT[ncn]])
                    p_h = [6, 7]

                    def wout_mm(u):
                        t = tb * 4 + u
                        s2 = t % 2
                        for half in range(2):
                            for c in range(8):
                                fw.op("pe", lambda h: h.matmul(ps[p_h[half]][:], lhsT=mT[:, c, u * 128:(u + 1) * 128],
                                                               rhs=Wout[:, c, half * 512:(half + 1) * 512], start=(c == 0), stop=(c == 7)),
                                      reads=[b_W] + b_mT, writes=[b_ps[p_h[half]]], inc=(c == 7))
                        for half in range(2):
                            fw.op("dve", lambda h: h.tensor_tensor(out=hh_[s2][:, half * 512:(half + 1) * 512], in0=ps[p_h[half]][:],
                                                                   in1=xt[s2][:, half * 512:(half + 1) * 512], op=ALU.add),
                                  reads=[b_ps[p_h[half]], b_xt[s2]], writes=[b_hh[s2]])
                        fw.dma("sp", b_hh[s2], [lambda h: h.dma_start(out=hscr[t * 128:(t + 1) * 128, :], in_=hh_[s2][:])],
                               reads=[b_hh[s2]], writes=[b_h])
                        rms_rstd(None, ssD[:, t:t + 1], rsD[:, t:t + 1], b_ssD[t], hh_[s2][:], junk[:], b_hh[s2], b_junk, D)
                        fw.op("dve", lambda h: h.scalar_tensor_tensor(out=xn[s2][:], in0=hh_[s2][:], scalar=rsD[:, t:t + 1], in1=g2[:],
                                                                      op0=ALU.mult, op1=ALU.mult),
                              reads=[b_hh[s2], b_ssD[t], b_g2], writes=[b_xn[s2]])
                        if t + 1 < NT:
                            load_x(t + 1)

                    def wout_tr(u):
                        nonlocal_pc = pcD[0]
                        pcD[0] += 1
                        t = tb * 4 + u
                        s2 = t % 2
                        pb = nonlocal_pc % 2
                        psb = ps[pb][:].bitcast(BF16)
                        for c in range(8):
                            fw.op("pe", lambda h: h.transpose(out=psb[:, c * 128:(c + 1) * 128], in_=xn[s2][:, c * 128:(c + 1) * 128],
                                                              identity=ident[:]),
                                  reads=[b_xn[s2], b_const], writes=[b_ps[pb]])
                        evac(xnTb[sl][:, :, u * 128:(u + 1) * 128], psb.rearrange("p (c j) -> p c j", c=8), [b_ps[pb]], [b_xnTb[sl]])

                    wout_mm(0)
                    for u in range(4):
                        if u + 1 < 4:
                            wout_mm(u + 1)
                        wout_tr(u)
                    dst = xn2T[:, tb * 512:(tb + 1) * 512].rearrange("(c p) j -> p c j", p=128)
                    fw.dma("sp", b_xnTb[sl], [lambda h: h.dma_start(out=dst, in_=xnTb[sl][:])], reads=[b_xnTb[sl]], writes=[b_xn2T])

        if "E" in phases:
            fw.barrier()
            with ExitStack() as st:
                Wq = sbt(st, "Wq", [128, 8, 2048], BF16)
                skb = sbt(st, "skb", [128, 16, 128], BF16)
                b_WE = fw.buf("WE", dma=True, stack=st, sw=True)
                fw.dma("pool", b_WE, [
                    lambda h: h.dma_start(out=Wq[:], in_=wq.rearrange("(c p) n -> p c n", p=128)),
                    lambda h: h.dma_start(out=skb[:], in_=skT),
                ], writes=[b_WE])
                xnb = [sbt(st, f"xnbE{i}", [128, 8, 512], BF16) for i in range(2)]
                b_xnb = fw.bufs(2, "xnbE", dma=True, stack=st)
                qTb2 = [sbt(st, f"qTbE{i}", [128, 16, 512], BF16) for i in range(2)]
                b_qTb2 = [fw.bufs(16, f"qTbE{i}_") for i in range(2)]
                sc = [sbt(st, f"sc{i}", [128, 16, 128], F32) for i in range(2)]
                b_sc = [fw.bufs(4, f"sc{i}_") for i in range(2)]
                work = sbt(st, "workE", [128, 16, 128], F32)
                b_work = fw.bufs(16, "workE")
                tops = sbt(st, "tops", [128, 16, 16], F32)
                idxs = sbt(st, "idxs", [128, 16, 16], U32)
                b_top = fw.bufs(16, "top")
                idxf = sbt(st, "idxf", [128, 16, 16], BF16)
                b_idxf = fw.buf("idxf")
                cand = sbt(st, "cand", [128, 8, 256], F32)
                b_cand = fw.buf("cand")
                work2 = sbt(st, "work2", [128, 8, 256], F32)
                b_work2 = fw.bufs(8, "work2")
                bests = sbt(st, "bests", [128, 8, 16], F32)
                pos = sbt(st, "pos", [128, 8, 16], U32)
                b_best = fw.bufs(8, "best")
                aru = sbt(st, "aru", [128, 8, 16], U32)
                bru = sbt(st, "bru", [128, 8, 16], U32)
                brf = sbt(st, "brf", [128, 8, 16], BF16)
                arf = sbt(st, "arf", [128, 8, 16], BF16)
                b_pf = fw.buf("pf")
                oha = sbt(st, "oha", [128, 8, 16, 16], BF16)
                ohb = sbt(st, "ohb", [128, 8, 16, 16], BF16)
                b_oha = fw.buf("oha")
                b_ohb = fw.buf("ohb")
                sel3 = sbt(st, "sel3", [128, 3, 128], BF16)
                b_sel3 = fw.bufs(3, "sel3")
                ee = sbt(st, "ee", [128, 8, 16], F32)
                esum = sbt(st, "esum", [128, 8, 1], F32)
                b_ee = fw.buf("ee")
                selT = [sbt(st, f"selT{i}", [128, 3, 512], BF16) for i in range(2)]
                b_selT = fw.bufs(2, "selT", dma=True, stack=st)
                sel_dst = (iaT, ibT, GT)

                def load_xnb(tb):
                    sl = tb % 2
                    fw.dma("sp", b_xnb[sl], [lambda h: h.dma_start(
                        out=xnb[sl][:], in_=xn2T[:, tb * 512:(tb + 1) * 512].rearrange("(c p) j -> p c j", p=128))],
                        reads=[b_xn2T], writes=[b_xnb[sl]])

                load_xnb(0)
                pc = 0
                iota16b = sbt(st, "iota16b", [128, 16], BF16)
                fw.op("dve", lambda h: h.tensor_copy(out=iota16b[:], in_=iota_f[:, 0:16]), reads=[b_const], writes=[b_const])
                iota16 = iota16b[:]
                npay = sbt(st, "npay", [128, 128], F32)
                fw.op("dve", lambda h: h.tensor_single_scalar(out=npay[:], in_=iota_f[:], scalar=2.0 ** -18, op=ALU.mult),
                      reads=[b_const], writes=[b_const])
                thr16 = sbt(st, "thr16", [128, 16], F32)
                fw.op("dve", lambda h: h.tensor_scalar(out=thr16[:], in0=iota16, scalar1=16.0, scalar2=16.0, op0=ALU.mult, op1=ALU.add),
                      reads=[b_const], writes=[b_const])
                qpc = [0]

                def emit_q(tbq, hps):
                    slq = tbq % 2
                    for hp in hps:
                        pb = qpc[0] % 4
                        qpc[0] += 1
                        for c in range(8):
                            fw.op("pe", lambda h: h.matmul(ps[pb][:], lhsT=Wq[:, c, hp * 128:(hp + 1) * 128], rhs=xnb[slq][:, c, :],
                                                           start=(c == 0), stop=(c == 7)),
                                  reads=[b_WE, b_xnb[slq]], writes=[b_ps[pb]], inc=(c == 7))
                        fw.op("act", lambda h: h.activation(out=qTb2[slq][:, hp, :], in_=ps[pb][:], func=AF.Copy),
                              reads=[b_ps[pb]], writes=[b_qTb2[slq][hp]])

                def emit_scores(t):
                    tb_, u_ = divmod(t, 4)
                    s2_ = t % 2
                    if "F" in phases and t < 16:
                        conv_piece(t)
                    if u_ == 0 and tb_ + 1 < NB5:
                        load_xnb(tb_ + 1)
                    if tb_ + 1 < NB5:
                        emit_q(tb_ + 1, range(4 * u_, 4 * u_ + 4))
                    for j in range(4):
                        for k in range(4):
                            hp = 4 * j + k
                            fw.op("pe", lambda h: h.matmul(ps[4 + j][:, k * 128:(k + 1) * 128], lhsT=qTb2[tb_ % 2][:, hp, u_ * 128:(u_ + 1) * 128],
                                                           rhs=skb[:, hp, :], start=True, stop=True),
                                  reads=[b_qTb2[tb_ % 2][hp], b_WE], writes=[b_ps[4 + j]])
                        fw.op("act", lambda h: h.activation(out=sc[s2_][:, 4 * j:4 * j + 4, :], in_=ps[4 + j][:].rearrange("p (k n) -> p k n", k=4),
                                                            func=AF.Identity, bias=6144.0, scale=1.0), reads=[b_ps[4 + j]], writes=[b_sc[s2_][j]])

                emit_q(0, range(16))
                emit_scores(0)
                for tb in range(NB5):
                    sl = tb % 2
                    for u in range(4):
                        t = tb * 4 + u
                        s2 = t % 2
                        if t + 1 < NT:
                            emit_scores(t + 1)
                        fw.op("dve", lambda h: h.scalar_tensor_tensor(out=sc[s2][:], in0=sc[s2][:], scalar=-6096.0,
                                                                      in1=npay[:].unsqueeze(1).to_broadcast([128, 16, 128]),
                                                                      op0=ALU.add, op1=ALU.add),
                              reads=b_sc[s2] + [b_const], writes=b_sc[s2])
                        for hp in range(16):
                            fw.op("dve", lambda h: h.max(out=tops[:, hp, 0:8], in_=sc[s2][:, hp, :]),
                                  reads=[b_sc[s2][hp // 4]], writes=[b_top[hp]])
                        for hp in range(16):
                            fw.op("dve", lambda h: h.match_replace(out=work[:, hp, :], in_to_replace=tops[:, hp, 0:8],
                                                                   in_values=sc[s2][:, hp, :], imm_value=NEG),
                                  reads=[b_sc[s2][hp // 4], b_top[hp]], writes=[b_work[hp]])
                        for hp in range(16):
                            fw.op("dve", lambda h: h.max(out=tops[:, hp, 8:16], in_=work[:, hp, :]),
                                  reads=[b_work[hp]], writes=[b_top[hp]])
                        fw.op("dve", lambda h: h.tensor_single_scalar(out=idxs[:], in_=tops[:].bitcast(U32), scalar=127, op=ALU.bitwise_and),
                              reads=b_top, writes=b_top)
                        fw.op("dve", lambda h: h.tensor_copy(out=idxf[:], in_=idxs[:]), reads=b_top, writes=[b_idxf])
                        tops4 = tops[:].rearrange("p (h two) k -> p h two k", two=2)
                        idxf4 = idxf[:].rearrange("p (h two) k -> p h two k", two=2)
                        fw.op("dve", lambda h: h.tensor_tensor(out=cand[:].rearrange("p h (a b) -> p h a b", a=16),
                                                               in0=tops4[:, :, 0, :].unsqueeze(3).to_broadcast([128, 8, 16, 16]),
                                                               in1=tops4[:, :, 1, :].unsqueeze(2).to_broadcast([128, 8, 16, 16]), op=ALU.add),
                              reads=b_top, writes=[b_cand])
                        for hd in range(8):
                            fw.op("dve", lambda h: h.max(out=bests[:, hd, 0:8], in_=cand[:, hd, :]), reads=[b_cand], writes=[b_best[hd]])
                        for hd in range(8):
                            fw.op("dve", lambda h: h.max_index(out=pos[:, hd, 0:8], in_max=bests[:, hd, 0:8], in_values=cand[:, hd, :]),
                                  reads=[b_cand], writes=[b_best[hd]])
                        for hd in range(8):
                            fw.op("dve", lambda h: h.match_replace(out=work2[:, hd, :], in_to_replace=bests[:, hd, 0:8],
                                                                   in_values=cand[:, hd, :], imm_value=NEG),
                                  reads=[b_cand, b_best[hd]], writes=[b_work2[hd]])
                        for hd in range(8):
                            fw.op("dve", lambda h: h.max(out=bests[:, hd, 8:16], in_=work2[:, hd, :]), reads=[b_work2[hd]], writes=[b_best[hd]])
                        for hd in range(8):
                            fw.op("dve", lambda h: h.max_index(out=pos[:, hd, 8:16], in_max=bests[:, hd, 8:16], in_values=work2[:, hd, :]),
                                  reads=[b_work2[hd]], writes=[b_best[hd]])
                        fw.op("dve", lambda h: h.tensor_single_scalar(out=aru[:], in_=pos[:], scalar=4, op=ALU.logical_shift_right),
                              reads=b_best, writes=[b_pf])
                        fw.op("dve", lambda h: h.tensor_single_scalar(out=bru[:], in_=pos[:], scalar=15, op=ALU.bitwise_and),
                              reads=b_best, writes=[b_pf])
                        fw.op("dve", lambda h: h.tensor_copy(out=arf[:], in_=aru[:]), reads=[b_pf], writes=[b_pf])
                        fw.op("dve", lambda h: h.tensor_copy(out=brf[:], in_=bru[:]), reads=[b_pf], writes=[b_pf])
                        io4 = iota16.unsqueeze(1).unsqueeze(1).to_broadcast([128, 8, 16, 16])
                        for which, (oh, b_oh, rk) in enumerate(((oha, b_oha, arf), (ohb, b_ohb, brf))):
                            fw.op("dve", lambda h: h.tensor_tensor(out=oh[:], in0=io4, in1=rk[:].unsqueeze(3).to_broadcast([128, 8, 16, 16]),
                                                                 op=ALU.is_equal), reads=[b_pf, b_const], writes=[b_oh])
                            fw.op("dve", lambda h: h.tensor_tensor(out=oh[:], in0=oh[:],
                                                                 in1=idxf4[:, :, which, :].unsqueeze(2).to_broadcast([128, 8, 16, 16]),
                                                                 op=ALU.mult), reads=[b_oh, b_idxf], writes=[b_oh])
                            fw.op("dve", lambda h: h.tensor_reduce(out=sel3[:, which, :].rearrange("p (h k) -> p h k", h=8), in_=oh[:],
                                                                   axis=AX.X, op=ALU.add), reads=[b_oh], writes=[b_sel3[which]])
                        fw.op("dve", lambda h: h.tensor_tensor(out=ee[:], in0=bests[:], in1=bests[:, :, 0:1].to_broadcast([128, 8, 16]),
                                                               op=ALU.subtract), reads=b_best, writes=[b_ee])
                        fw.op("act", lambda h: h.activation(out=ee[:], in_=ee[:], func=AF.Exp), reads=[b_ee], writes=[b_ee])
                        fw.op("dve", lambda h: h.tensor_reduce(out=esum[:], in_=ee[:], axis=AX.X, op=ALU.add), reads=[b_ee], writes=[b_ee])
                        fw.op("dve", lambda h: h.reciprocal(out=esum[:], in_=esum[:]), reads=[b_ee], writes=[b_ee])
                        fw.op("dve", lambda h: h.tensor_tensor(out=sel3[:, 2, :].rearrange("p (h k) -> p h k", h=8), in0=ee[:],
                                                               in1=esum[:].to_broadcast([128, 8, 16]), op=ALU.mult),
                              reads=[b_ee], writes=[b_sel3[2]])
                        for k3 in range(3):
                            pb = qpc[0] % 4
                            qpc[0] += 1
                            psb = ps[pb][:].bitcast(BF16)
                            fw.op("pe", lambda h: h.transpose(out=psb[:, 0:128], in_=sel3[:, k3, :], identity=ident[:]),
                                  reads=[b_sel3[k3], b_const], writes=[b_ps[pb]])
                            fw.op("act", lambda h: h.activation(out=selT[sl][:, k3, u * 128:(u + 1) * 128], in_=psb[:, 0:128], func=AF.Copy),
                                  reads=[b_ps[pb]], writes=[b_selT[sl]])
                    fw.dma("sp", b_selT[sl], [(lambda h, k3=k3: h.dma_start(out=sel_dst[k3][:, tb * 512:(tb + 1) * 512], in_=selT[sl][:, k3, :]))
                                              for k3 in range(3)], reads=[b_selT[sl]], writes=[b_sel])

        if "F" in phases:
            fw.barrier()
            with ExitStack() as st:
                TB = 256
                NBK = S // TB
                Wall = [sbt(st, f"Wall{i}", [128, TB, 128], BF16) for i in range(2)]
                b_Wall = fw.bufs(2, "Wall")
                NUV = 4
                Uc = [sbt(st, f"Uc{i}", [128, 2, 8, 128], BF16) for i in range(NUV)]
                Vc = [sbt(st, f"Vc{i}", [128, 2, 1024], BF16) for i in range(NUV)]
                b_Uc = fw.bufs(NUV, "Uc", dma=True, stack=st)
                b_Vc = fw.bufs(NUV, "Vc", dma=True, stack=st)
                hT = sbt(st, "hT", [128, 2, D], F32)
                b_hT = fw.bufs(2, "hT", dma=True, stack=st)
                xnF = [sbt(st, f"xnF{i}", [128, 8, TB], BF16) for i in range(2)]
                selF = [sbt(st, f"selF{i}", [128, 3, TB], BF16) for i in range(2)]
                b_ldF = fw.bufs(2, "ldF", dma=True, stack=st)
                gf = sbt(st, "gf", [128, D], F32)
                b_gf = fw.buf("gf", dma=True, stack=st)
                fw.dma("sp", b_gf, [lambda h: h.dma_start(out=gf[:], in_=gfb)], writes=[b_gf])
                iota_b = sbt(st, "iota_b", [128, 128], BF16)
                fw.op("dve", lambda h: h.tensor_copy(out=iota_b[:], in_=iota_f[:]), reads=[b_const], writes=[b_const])
                NSL = 3
                OA = [sbt(st, f"OA{i}", [128, 4, 128], BF16) for i in range(NSL)]
                OG = [sbt(st, f"OG{i}", [128, 4, 128], BF16) for i in range(NSL)]
                b_OA = fw.bufs(NSL, "OA")
                b_OG = fw.bufs(NSL, "OG")
                b_OGa = fw.bufs(NSL, "OGa")
                selF32 = sbt(st, "selF32", [128, 3, TB], F32)
                negs = sbt(st, "negs", [128, 2, TB], F32)
                tmpA = [sbt(st, f"tmpA{i}", [128, 128], F32) for i in range(2)]
                b_tmpA = fw.bufs(2, "tmpA")
                b_sel32 = fw.buf("sel32")
                Gl = [sbt(st, f"Gl{i}", [128, TB], BF16) for i in range(3)]
                Wg = [sbt(st, f"Wg{i}", [128, TB], BF16) for i in range(3)]
                b_Gl = fw.bufs(3, "Gl")
                b_Wg = fw.bufs(3, "Wg")
                junk = sbt(st, "junkF", [128, D], BF16)
                b_junk = fw.buf("junkF")
                ssF = sbt(st, "ssF", [128, NT], F32)
                rsF = sbt(st, "rsF", [128, NT], F32)
                b_ssF = fw.bufs(NT, "ssF")
                sel_src = (iaT, ibT, GT)

                def load_blkF(bk):
                    sl = bk % 2
                    cs = slice(bk * TB, (bk + 1) * TB)
                    fns = [lambda h: h.dma_start(out=xnF[sl][:], in_=xn2T[:, cs].rearrange("(c p) j -> p c j", p=128))]
                    for k3 in range(3):
                        fns.append(lambda h, k3=k3: h.dma_start(out=selF[sl][:, k3, :], in_=sel_src[k3][:, cs]))
                    fw.dma("sp", b_ldF[sl], fns, reads=[b_xn2T, b_sel], writes=[b_ldF[sl]])

                def load_h(bk):
                    for sub in range(2):
                        rows = slice(bk * TB + sub * 128, bk * TB + (sub + 1) * 128)
                        fw.dma("sp", b_hT[sub], [lambda h: h.dma_start(out=hT[:, sub, :], in_=hscr[rows, :])], reads=[b_h], writes=[b_hT[sub]])

                NG = 64

                def load_uv(gi):
                    ag = gi % NG
                    s4 = gi % NUV
                    rows = slice(ag * 256, (ag + 1) * 256)
                    fw.dma("sp", b_Uc[s4], [lambda h: h.dma_start(out=Uc[s4][:], in_=ubf[rows, :].rearrange("(a p) (c b) -> p a c b", p=128, c=8))],
                           reads=[b_ubf], writes=[b_Uc[s4]])
                    fw.dma("sp", b_Vc[s4], [lambda h: h.dma_start(out=Vc[s4][:], in_=vbf[rows, :].rearrange("(a b) n -> b a n", b=128))],
                           reads=[b_vbf], writes=[b_Vc[s4]])

                def prep_block(bk):
                    sl = bk % 2
                    fw.op("dve", lambda h: h.tensor_copy(out=selF32[:], in_=selF[sl][:]), reads=[b_ldF[sl]], writes=[b_sel32])
                    fw.op("dve", lambda h: h.tensor_single_scalar(out=negs[:], in_=selF32[:, 1:3, :], scalar=-1.0, op=ALU.mult),
                          reads=[b_sel32], writes=[b_sel32])

                osl = [0]
                acnt = [0]

                def build_steps(bk):
                    x = bk % 2
                    NGR = TB // 4
                    slots = {}

                    def onehots(g):
                        w = osl[0] % NSL
                        osl[0] += 1
                        slots[g] = w
                        for tt_ in range(4):
                            t = g * 4 + tt_
                            fw.op("dve", lambda h: h.tensor_scalar(out=OA[w][:, tt_, :], in0=iota_b[:], scalar1=selF32[:, 0, t:t + 1],
                                                                   scalar2=None, op0=ALU.is_equal),
                                  reads=[b_sel32, b_const], writes=[b_OA[w]])
                            if tt_ == 0:
                                ta = acnt[0] % 2
                                acnt[0] += 1
                                fw.op("act", lambda h: h.activation(out=tmpA[ta][:], in_=iota_f[:], func=AF.Square, bias=negs[:, 0, t:t + 1], scale=1.0),
                                      reads=[b_sel32, b_const], writes=[b_tmpA[ta]])
                                fw.op("act", lambda h: h.activation(out=OG[w][:, tt_, :], in_=tmpA[ta][:], func=AF.Relu, bias=selF32[:, 2, t:t + 1],
                                                                    scale=negs[:, 1, t:t + 1]),
                                      reads=[b_sel32, b_tmpA[ta]], writes=[b_OGa[w]])
                            else:
                                fw.op("dve", lambda h: h.tensor_scalar(out=OG[w][:, tt_, :], in0=iota_b[:], scalar1=selF32[:, 1, t:t + 1],
                                                                       scalar2=selF32[:, 2, t:t + 1], op0=ALU.is_equal, op1=ALU.mult),
                                      reads=[b_sel32, b_const], writes=[b_OG[w]])

                    def wmm(g):
                        w = slots[g]
                        pb = 6 + g % 2
                        for tt_ in range(4):
                            fw.op("pe", lambda h: h.matmul(ps[pb][:, tt_ * 128:(tt_ + 1) * 128], lhsT=OG[w][:, tt_, :], rhs=OA[w][:, tt_, :],
                                                           start=True, stop=True), reads=[b_OG[w], b_OGa[w], b_OA[w]], writes=[b_ps[pb]], inc=(tt_ == 3))
                        t4 = slice(g * 4, g * 4 + 4)
                        fw.op("act", lambda h: h.activation(out=Wall[x][:, t4, :], in_=ps[pb][:].rearrange("p (t a) -> p t a", t=4), func=AF.Copy),
                              reads=[b_ps[pb]], writes=[b_Wall[x]])

                    LAG = 1
                    steps = []
                    for g in range(NGR + LAG):
                        fns = []
                        if g - LAG >= 0:
                            fns.append(lambda g=g: wmm(g - LAG))
                        if g < NGR:
                            fns.append(lambda g=g: onehots(g))
                        steps.append(lambda fns=fns: [f() for f in fns])
                    return steps

                load_blkF(0)
                for gi0 in range(NUV - 1):
                    load_uv(gi0)
                prep_block(0)
                for stp in build_steps(0):
                    stp()
                for bk in range(NBK):
                    sl = bk % 2
                    x = bk % 2
                    if bk + 1 < NBK:
                        load_blkF(bk + 1)
                    load_h(bk)
                    pending = []
                    if bk + 1 < NBK:
                        prep_block(bk + 1)
                        pending = build_steps(bk + 1)
                    nsteps = len(pending)
                    done_steps = 0

                    def emit_H(a, s4, aa):
                        e2 = a % 3
                        ph = 4 + a % 2
                        for c in range(8):
                            fw.op("pe", lambda h: h.matmul(ps[ph][:, 0:TB], lhsT=Uc[s4][:, aa, c, :], rhs=xnF[sl][:, c, :],
                                                           start=(c == 0), stop=(c == 7)),
                                  reads=[b_Uc[s4], b_ldF[sl]], writes=[b_ps[ph]], inc=(c == 7))
                        fw.op("act", lambda h: h.activation(out=Gl[e2][:], in_=ps[ph][:, 0:TB], func=AF.Gelu),
                              reads=[b_ps[ph]], writes=[b_Gl[e2]])
                        fw.op("pool", lambda h: h.tensor_tensor(out=Wg[e2][:], in0=Gl[e2][:], in1=Wall[x][:, :, a], op=ALU.mult),
                              reads=[b_Gl[e2], b_Wall[x]], writes=[b_Wg[e2]])

                    def emit_V(a, s4, aa):
                        e2 = a % 3
                        for sub in range(2):
                            for half in range(2):
                                po = sub * 2 + half
                                fw.op("pe", lambda h: h.matmul(ps[po][:], lhsT=Wg[e2][:, sub * 128:(sub + 1) * 128],
                                                               rhs=Vc[s4][:, aa, half * 512:(half + 1) * 512], start=(a == 0), stop=(a == 127)),
                                      reads=[b_Wg[e2], b_Vc[s4]], writes=[b_ps[po]], inc=(po == 3))

                    g0 = bk * NG

                    def uv_slot(a):
                        return (g0 + a // 2) % NUV

                    emit_H(0, uv_slot(0), 0)
                    emit_H(1, uv_slot(1), 1)
                    for a in range(128):
                        ag, aa = divmod(a, 2)
                        if aa == 0 and g0 + ag + NUV - 1 < NBK * NG:
                            load_uv(g0 + ag + NUV - 1)
                        if a + 2 < 128:
                            emit_H(a + 2, uv_slot(a + 2), (a + 2) % 2)
                        want = min(nsteps, (a + 1) * nsteps // 120)
                        while done_steps < want and pending:
                            pending.pop(0)()
                            done_steps += 1
                        emit_V(a, uv_slot(a), aa)
                    while pending:
                        pending.pop(0)()
                    for sub in range(2):
                        t = bk * 2 + sub
                        for half in range(2):
                            po = sub * 2 + half
                            fw.op("dve", lambda h: h.tensor_tensor(out=hT[:, sub, half * 512:(half + 1) * 512], in0=ps[po][:],
                                                                   in1=hT[:, sub, half * 512:(half + 1) * 512], op=ALU.add),
                                  reads=[b_ps[po], b_hT[sub]], writes=[b_hT[sub]])
                        rms_rstd(None, ssF[:, t:t + 1], rsF[:, t:t + 1], b_ssF[t], hT[:, sub, :], junk[:], b_hT[sub], b_junk, D)
                        fw.op("dve", lambda h: h.scalar_tensor_tensor(out=hT[:, sub, :], in0=hT[:, sub, :], scalar=rsF[:, t:t + 1], in1=gf[:],
                                                                      op0=ALU.mult, op1=ALU.mult),
                              reads=[b_ssF[t], b_gf], writes=[b_hT[sub]])
                        fw.dma("sp", b_hT[sub], [lambda h: h.dma_start(out=out[t * 128:(t + 1) * 128, :], in_=hT[:, sub, :])],
                               reads=[b_hT[sub]], writes=[b_out])

        fw.finish([b_fm, b_tm, b_retT, b_og, b_h, b_xn2T, b_sel, b_out, b_ubf, b_vbf])
        nc._fw_ninst = fw.ninst
    return nc


def make_in_maps(inputs, S=4096):
    f = lambda a: np.ascontiguousarray(np.asarray(a, dtype=np.float32))
    x = f(inputs["x"])
    dt_ret, wq_ret, wk_ret, _, eb = _constants()
    rep = lambda v: np.ascontiguousarray(np.broadcast_to(f(v).reshape(1, D), (128, D)))
    u = f(inputs["peer_u"])[0].reshape(128, 128, 8, 128)
    common = {
        "w_in": f(inputs["w_in"])[0],
        "g1b": rep(inputs["norm1_g"][0]),
        "g2b": rep(inputs["norm2_g"][0]),
        "gfb": rep(inputs["normf_g"]),
        "w_ret": f(inputs["w_ret_out"])[0],
        "w_att": f(inputs["w_att_out"])[0],
        "w_out": f(inputs["w_out"])[0],
        "wq": f(inputs["peer_wq"])[0],
        "skT": np.ascontiguousarray(f(inputs["peer_subkeys"])[0].transpose(3, 0, 1, 2).reshape(128, 16, 128)),
        "ut": np.ascontiguousarray(u.transpose(0, 3, 2, 1).reshape(16384, 1024)),
        "vt": f(inputs["peer_v"])[0],
        "c_dt": dt_ret, "c_wq": wq_ret, "c_wk": wk_ret, "c_eb": eb,
    }
    return [dict(common, x=np.ascontiguousarray(x[b, :S])) for b in range(x.shape[0])]


def kernel(**inputs):
    S = 4096
    nc = build_nc(S=S)
    maps = make_in_maps(inputs, S=S)
    res = run_bass_kernel_spmd(nc, maps, core_ids=list(range(8)))
    return np.stack([np.asarray(r["out"], dtype=np.float32) for r in res.results], axis=0)
```

```python
import numpy as np
from contextlib import ExitStack
import concourse.bass as bass
import concourse.mybir as mybir
from concourse.bass_utils import run_bass_kernel_spmd

F32 = mybir.dt.float32
BF16 = mybir.dt.bfloat16
U32 = mybir.dt.uint32
ALU = mybir.AluOpType
AF = mybir.ActivationFunctionType
AX = mybir.AxisListType

D = 1024
IN_W = 9728
EPS = 1e-6
NEG = -1e30


class Buf:
    __slots__ = ("name", "w", "r", "dsem", "dcount", "wl")

    def __init__(self, name, multi=False):
        self.name = name
        self.w = None
        self.r = []
        self.dsem = None
        self.dcount = 0
        self.wl = {} if multi else None


class Eng:
    def __init__(self, name, handle, sem):
        self.name = name
        self.h = handle
        self.sem = sem
        self.count = 0
        self.seen = {}


class FW:
    def __init__(self, nc, stack):
        self.nc = nc
        self.stack = stack
        self.engs = {}
        for name, h in (("pe", nc.tensor), ("act", nc.scalar), ("dve", nc.vector),
                        ("pool", nc.gpsimd), ("sp", nc.sync)):
            sem = stack.enter_context(nc.semaphore("sem_" + name))
            self.engs[name] = Eng(name, h, sem)
        self.nbuf = 0
        self.ninst = 0
        self.free_sems = []
        self.free_sems_sw = []
        self.dma_counts = {}
        self.no_barrier = set()

    def buf(self, name=None, dma=False, stack=None, multi=False, sw=False):
        self.nbuf += 1
        b = Buf(name or f"b{self.nbuf}", multi)
        if dma:
            pool = self.free_sems_sw if sw else self.free_sems
            if pool:
                b.dsem, b.dcount = pool.pop()
            else:
                b.dsem = self.stack.enter_context(self.nc.semaphore(f"ds{self.nbuf}"))
            if stack is not None:
                stack.callback(self._release, b, pool)
        return b

    def _release(self, b, pool):
        pool.append((b.dsem, b.dcount))

    def bufs(self, n, name, dma=False, stack=None, sw=False):
        return [self.buf(f"{name}{i}", dma, stack, sw=sw) for i in range(n)]

    def _waits(self, E, reads, writes, extra=()):
        waits = {}

        def need(ev):
            if ev is None:
                return
            sem, val, src = ev
            if src == "pe" and E.name == "pe":
                return
            k = id(sem)
            if E.seen.get(k, 0) >= val:
                return
            if src == E.name:
                val = E.count
            if k not in waits or waits[k][1] < val:
                waits[k] = (sem, val)

        for b in reads:
            if b.wl is not None:
                for ev in b.wl.values():
                    need(ev)
            else:
                need(b.w)
        for b in writes:
            if b.wl is not None:
                continue
            need(b.w)
            for ev in b.r:
                need(ev)
        for ev in extra:
            need(ev)
        for k, (sem, val) in waits.items():
            E.h.wait_ge(sem, val)
            E.seen[k] = val
            self.ninst += 1

    def op(self, eng, fn, reads=(), writes=(), inc=True):
        E = self.engs[eng]
        self._waits(E, reads, writes)
        ins = fn(E.h)
        self.ninst += 1
        if inc:
            E.count += 1
            ins.then_inc(E.sem, 1)
            ev = (E.sem, E.count, eng)
        else:
            assert eng == "pe"
            ev = (E.sem, E.count + 1, eng)
        self._record(ev, reads, writes)
        return ev

    def _record(self, ev, reads, writes):
        for b in reads:
            if b.wl is not None:
                continue
            b.r = [e for e in b.r if e[0] is not ev[0]]
            b.r.append(ev)
        for b in writes:
            if b.wl is not None:
                b.wl[id(ev[0])] = ev
            else:
                b.w = ev
                b.r = []

    def dma(self, eng, owner, fns, reads=(), writes=()):
        E = self.engs[eng]
        extra = []
        if owner.dcount > 0:
            extra.append((owner.dsem, owner.dcount, "dma"))
        self._waits(E, reads, writes, extra)
        for fn in fns:
            ins = fn(E.h)
            owner.dcount += 16
            ins.then_inc(owner.dsem, 16)
            self.ninst += 1
        self.dma_counts[id(owner.dsem)] = (owner.dsem, owner.dcount)
        ev = (owner.dsem, owner.dcount, "dma")
        self._record(ev, reads, writes)
        return ev

    def barrier(self):
        evs = [(E.sem, E.count, "x") for E in self.engs.values() if E.count > 0]
        evs += [(sem, cnt, "dma") for k, (sem, cnt) in self.dma_counts.items() if cnt > 0 and k not in self.no_barrier]
        for E in self.engs.values():
            self._waits(E, (), (), evs)

    def finish(self, bufs):
        self.no_barrier = set()
        self.barrier()


def _constants():
    H = 8
    log_g = np.log1p(-np.exp2(-5.0 - np.arange(H, dtype=np.float64)))
    pos = np.arange(128, dtype=np.float64)
    diff = pos[None, :] - pos[:, None]
    dt_ret = np.where(diff[:, None, :] >= 0, np.exp(np.maximum(diff, 0)[:, None, :] * log_g[None, :, None]), 0.0) / 8.0
    wq_ret = np.exp((pos + 1)[None, None, :] * log_g[None, :, None]) * np.ones((64, 1, 1))
    wk_ret = np.exp((127 - pos)[:, None, None] * log_g[None, :, None]) / 8.0 * np.ones((1, 1, 64))
    g128 = np.exp(128 * log_g)
    slopes = np.exp2(-8.0 * np.arange(1, 13, dtype=np.float64) / 12)
    dil = np.repeat(np.array([1.0, 4.0, 16.0]), 4)
    k = pos[:, None]
    q = pos[None, :]
    eb = np.zeros((128, 12, 2, 128))
    for h in range(12):
        dprev = q - k + 128
        dcur = q - k
        eb[:, h, 0, :] = np.where(k >= q, np.exp(-slopes[h] * dil[h] * dprev), 0.0)
        eb[:, h, 1, :] = np.where(k <= q, np.exp(-slopes[h] * dil[h] * dcur), 0.0)
    return (dt_ret.astype(np.float32), wq_ret.astype(np.float32), wk_ret.astype(np.float32),
            [float(v) for v in g128], eb.astype(np.float32))


FM_RQ, FM_RK, FM_RG, FM_AQ, FM_AK, FM_GR, FM_GA = 0, 512, 1024, 2048, 3584, 5120, 6144
FM_ROWS = 7168
TM_RK, TM_RV, TM_AV = 0, 512, 1536
TM_COLS = 3072


def _slices():
    sl = []
    sl.append(("fm", 0, FM_RQ))
    sl.append(("fm", 512, FM_RK))
    for i in range(2):
        sl.append(("fm", 2048 + 512 * i, FM_RG + 512 * i))
    for i in range(3):
        sl.append(("fm", 3072 + 512 * i, FM_AQ + 512 * i))
    for i in range(3):
        sl.append(("fm", 4608 + 512 * i, FM_AK + 512 * i))
    for i in range(2):
        sl.append(("fm", 7680 + 512 * i, FM_GR + 512 * i))
    for i in range(2):
        sl.append(("fm", 8704 + 512 * i, FM_GA + 512 * i))
    sl.append(("tm", 512, TM_RK))
    for i in range(2):
        sl.append(("tm", 1024 + 512 * i, TM_RV + 512 * i))
    for i in range(3):
        sl.append(("tm", 6144 + 512 * i, TM_AV + 512 * i))
    return sl


def build_nc(S=4096, phases="ABCDEF", debug=False):
    NT = S // 128
    NB5 = S // 512
    dt_ret_np, wq_ret_np, wk_ret_np, G128, eb_np = _constants()
    nc = bass.Bass("TRN2", target_bir_lowering=False)
    dkind = "ExternalOutput" if debug else "Internal"

    def din(name, shape, dt=F32):
        return nc.dram_tensor(name, list(shape), dt, kind="ExternalInput").ap()

    x = din("x", [S, D])
    w_in = din("w_in", [D, IN_W])
    g1b = din("g1b", [128, D])
    g2b = din("g2b", [128, D])
    gfb = din("gfb", [128, D])
    w_ret = din("w_ret", [1024, D])
    w_att = din("w_att", [512, D])
    w_out = din("w_out", [D, D])
    wq = din("wq", [D, 2048])
    skT = din("skT", [128, 16, 128])
    ut = din("ut", [16384, 1024])
    vt = din("vt", [16384, 1024])
    c_dt = din("c_dt", [128, 8, 128])
    c_wq = din("c_wq", [64, 8, 128])
    c_wk = din("c_wk", [128, 8, 64])
    c_eb = din("c_eb", [128, 12, 2, 128])
    out = nc.dram_tensor("out", [S, D], F32, kind="ExternalOutput").ap()

    fm = nc.dram_tensor("fm", [FM_ROWS, S], BF16, kind=dkind).ap()
    tm = nc.dram_tensor("tm", [S, TM_COLS], BF16, kind=dkind).ap()
    retT = nc.dram_tensor("retT", [1024, S], BF16, kind=dkind).ap()
    og = nc.dram_tensor("og", [3, S, 4, 132], F32, kind=dkind).ap()
    hscr = nc.dram_tensor("hscr", [S, D], F32, kind=dkind).ap()
    xn2T = nc.dram_tensor("xn2T", [D, S], BF16, kind=dkind).ap()
    iaT = nc.dram_tensor("iaT", [128, S], BF16, kind=dkind).ap()
    ibT = nc.dram_tensor("ibT", [128, S], BF16, kind=dkind).ap()
    GT = nc.dram_tensor("GT", [128, S], BF16, kind=dkind).ap()
    ubf = nc.dram_tensor("ubf", [16384, 1024], BF16, kind="Internal").ap()
    vbf = nc.dram_tensor("vbf", [16384, 1024], BF16, kind="Internal").ap()

    with ExitStack() as top:
        top.enter_context(nc.allow_low_precision("one-hot index decode sums are exact small integers; gates are bf16 matmul operands"))
        fw = FW(nc, top)

        def sbt(st, name, shape, dt):
            return st.enter_context(nc.sbuf_tensor(name, list(shape), dt))

        ps = [top.enter_context(nc.psum_tensor(f"ps{i}", [128, 512], F32)) for i in range(8)]
        b_ps = fw.bufs(8, "ps")
        b_fm = fw.buf("fm", multi=True)
        b_tm = fw.buf("tm", multi=True)
        b_retT = fw.buf("retT", multi=True)
        b_og = fw.buf("og", multi=True)
        b_h = fw.buf("hscr", multi=True)
        b_xn2T = fw.buf("xn2T", multi=True)
        b_sel = fw.buf("sel", multi=True)
        b_ubf = fw.buf("ubf", multi=True)
        b_vbf = fw.buf("vbf", multi=True)
        b_conv = fw.bufs(16, "conv", dma=True, sw=True)
        b_out = fw.buf("out", multi=True)
        fw.no_barrier = {id(b.dsem) for b in b_conv}

        def conv_piece(i):
            src, dst, tok = (ut, ubf, b_ubf) if i < 8 else (vt, vbf, b_vbf)
            r0 = (i % 8) * 2048
            fw.dma("pool", b_conv[i], [(lambda h, j=j: h.dma_start(out=dst[r0 + j * 1024:r0 + (j + 1) * 1024, :],
                                                                 in_=src[r0 + j * 1024:r0 + (j + 1) * 1024, :])) for j in range(2)],
                   writes=[tok])

        ident = sbt(top, "ident", [128, 128], BF16)
        identf = sbt(top, "identf", [128, 128], F32)
        iota_f = sbt(top, "iota_f", [128, 128], F32)
        b_const = fw.buf("const")
        fw.op("pool", lambda h: h.iota(identf[:], pattern=[[1, 128]], base=0, channel_multiplier=-1,
                                       allow_small_or_imprecise_dtypes=True), writes=[b_const])
        fw.op("dve", lambda h: h.tensor_single_scalar(out=ident[:], in_=identf[:], scalar=0.0, op=ALU.is_equal),
              reads=[b_const], writes=[b_const])
        fw.op("dve", lambda h: h.tensor_single_scalar(out=identf[:], in_=identf[:], scalar=0.0, op=ALU.is_equal),
              reads=[b_const], writes=[b_const])
        fw.op("pool", lambda h: h.iota(iota_f[:], pattern=[[1, 128]], base=0, channel_multiplier=0,
                                       allow_small_or_imprecise_dtypes=True), writes=[b_const])

        evac_ctr = [0]

        def evac(out_ap, in_ap, reads, writes):
            evac_ctr[0] += 1
            if evac_ctr[0] % 2:
                fw.op("act", lambda h: h.activation(out=out_ap, in_=in_ap, func=AF.Copy), reads=reads, writes=writes)
            else:
                fw.op("dve", lambda h: h.tensor_copy(out=out_ap, in_=in_ap), reads=reads, writes=writes)

        def rms_rstd(st_tile, ss_ap, rstd_ap, b_ss, src_ap, junk_ap, b_src, b_junk, n):
            fw.op("act", lambda h: h.activation(out=junk_ap, in_=src_ap, func=AF.Square, accum_out=ss_ap),
                  reads=[b_src], writes=[b_junk, b_ss])
            fw.op("act", lambda h: h.activation(out=rstd_ap, in_=ss_ap, func=AF.Sqrt, bias=EPS, scale=1.0 / n),
                  reads=[b_ss], writes=[b_ss])
            fw.op("dve", lambda h: h.reciprocal(out=rstd_ap, in_=rstd_ap), reads=[b_ss], writes=[b_ss])

        if "A" in phases:
            with ExitStack() as st:
                xT = sbt(st, "xT", [128, 8, S], BF16)
                b_xT = fw.bufs(NT, "xT")
                xin = [sbt(st, f"xin{i}", [128, D], F32) for i in range(3)]
                b_xin = fw.bufs(3, "xin", dma=True, stack=st)
                xb = [sbt(st, f"xb{i}", [128, D], BF16) for i in range(2)]
                b_xb = fw.bufs(2, "xb")
                junk = sbt(st, "junkA", [128, D], BF16)
                b_junk = fw.buf("junkA")
                g1 = sbt(st, "g1", [128, D], F32)
                b_g1 = fw.buf("g1", dma=True, stack=st)
                ssA = sbt(st, "ssA", [128, NT], F32)
                rsA = sbt(st, "rsA", [128, NT], F32)
                b_ssA = fw.bufs(NT, "ssA")
                wbf = [sbt(st, f"wbf{i}", [128, 8, 512], BF16) for i in range(2)]
                b_wbf = fw.bufs(2, "wbf", dma=True, stack=st, sw=True)
                ost = [sbt(st, f"ostA{i}", [128, 4, 512], BF16) for i in range(2)]
                b_ost = fw.bufs(2, "ostA", dma=True, stack=st)

                fw.dma("sp", b_g1, [lambda h: h.dma_start(out=g1[:], in_=g1b)], writes=[b_g1])
                slices = _slices()

                def load_w(si):
                    kind, c0, d0 = slices[si]
                    sl = si % 2
                    src = w_in[:, c0:c0 + 512].rearrange("(c p) n -> p c n", p=128)
                    fw.dma("pool", b_wbf[sl], [lambda h: h.dma_start(out=wbf[sl][:], in_=src)], writes=[b_wbf[sl]])

                load_w(0)
                load_w(1)
                def prep_tile(t):
                    s3 = t % 3
                    s2 = t % 2
                    fw.dma("sp", b_xin[s3], [lambda h: h.dma_start(out=xin[s3][:], in_=x[t * 128:(t + 1) * 128, :])],
                           writes=[b_xin[s3]])
                    rms_rstd(None, ssA[:, t:t + 1], rsA[:, t:t + 1], b_ssA[t], xin[s3][:], junk[:], b_xin[s3], b_junk, D)
                    fw.op("dve", lambda h: h.scalar_tensor_tensor(out=xb[s2][:], in0=xin[s3][:], scalar=rsA[:, t:t + 1],
                                                                  in1=g1[:], op0=ALU.mult, op1=ALU.mult),
                          reads=[b_xin[s3], b_ssA[t], b_g1], writes=[b_xb[s2]])
                    pb = s2
                    psb = ps[pb][:].bitcast(BF16)
                    for c in range(8):
                        fw.op("pe", lambda h: h.transpose(out=psb[:, c * 128:(c + 1) * 128], in_=xb[s2][:, c * 128:(c + 1) * 128],
                                                          identity=ident[:]),
                              reads=[b_xb[s2], b_const], writes=[b_ps[pb]])
                    evac(xT[:, :, t * 128:(t + 1) * 128], psb.rearrange("p (c j) -> p c j", c=8), [b_ps[pb]], [b_xT[t]])

                for t in range(8):
                    prep_tile(t)
                pctr = 0
                octr = 0
                for si, (kind, c0, d0) in enumerate(slices):
                    sl = si % 2
                    if kind == "fm":
                        for tb in range(NB5):
                            if si == 0:
                                for t in range(4 * (tb + 2), min(NT, 4 * (tb + 3))):
                                    prep_tile(t)
                            os_ = octr % 2
                            octr += 1
                            for q in range(4):
                                pb = 2 + pctr % 6
                                pctr += 1
                                for c in range(8):
                                    fw.op("pe", lambda h: h.matmul(ps[pb][:], lhsT=wbf[sl][:, c, q * 128:(q + 1) * 128],
                                                                   rhs=xT[:, c, tb * 512:(tb + 1) * 512], start=(c == 0), stop=(c == 7)),
                                          reads=[b_wbf[sl]] + b_xT[tb * 4:tb * 4 + 4], writes=[b_ps[pb]], inc=(c == 7))
                                evac(ost[os_][:, q, :], ps[pb][:], [b_ps[pb]], [b_ost[os_]])
                            dst = fm[d0:d0 + 512, tb * 512:(tb + 1) * 512].rearrange("(q p) j -> p q j", p=128)
                            fw.dma("sp", b_ost[os_], [lambda h: h.dma_start(out=dst, in_=ost[os_][:])],
                                   reads=[b_ost[os_]], writes=[b_fm])
                    else:
                        for t4 in range(NT // 4):
                            os_ = octr % 2
                            octr += 1
                            for u in range(4):
                                t = t4 * 4 + u
                                pb = 2 + pctr % 6
                                pctr += 1
                                for c in range(8):
                                    fw.op("pe", lambda h: h.matmul(ps[pb][:], lhsT=xT[:, c, t * 128:(t + 1) * 128],
                                                                   rhs=wbf[sl][:, c, :], start=(c == 0), stop=(c == 7)),
                                          reads=[b_wbf[sl], b_xT[t]], writes=[b_ps[pb]], inc=(c == 7))
                                evac(ost[os_][:, u, :], ps[pb][:], [b_ps[pb]], [b_ost[os_]])
                            dst = tm[t4 * 512:(t4 + 1) * 512, d0:d0 + 512].rearrange("(u p) n -> p u n", p=128)
                            fw.dma("sp", b_ost[os_], [lambda h: h.dma_start(out=dst, in_=ost[os_][:])],
                                   reads=[b_ost[os_]], writes=[b_tm])
                    if si + 2 < len(slices):
                        load_w(si + 2)

        if "B" in phases:
            fw.barrier()
            with ExitStack() as st:
                qTb = [sbt(st, f"qTb{i}", [64, 8, 512], BF16) for i in range(2)]
                kTb = [sbt(st, f"kTb{i}", [64, 8, 512], BF16) for i in range(2)]
                gTb = [sbt(st, f"gTb{i}", [128, 8, 512], BF16) for i in range(2)]
                ktm = [sbt(st, f"ktm{i}", [128, 4, 512], BF16) for i in range(2)]
                vtm = [sbt(st, f"vtm{i}", [128, 4, 1024], BF16) for i in range(2)]
                b_ld = fw.bufs(2, "ldB", dma=True, stack=st)
                cdt = sbt(st, "cdt", [128, 8, 128], F32)
                cwq = sbt(st, "cwq", [64, 8, 128], F32)
                cwk = sbt(st, "cwk", [128, 8, 64], F32)
                b_cB = fw.buf("cB", dma=True, stack=st)
                ones = sbt(st, "ones", [128, 128], BF16)
                qs = [sbt(st, f"qs{i}", [64, 8, 128], BF16) for i in range(2)]
                b_qs = fw.bufs(2, "qs")
                kw = [sbt(st, f"kw{i}", [128, 512], BF16) for i in range(2)]
                b_kw = fw.bufs(2, "kw")
                A = [sbt(st, f"A{i}", [128, 4, 128], BF16) for i in range(2)]
                b_A = fw.bufs(2, "A")
                sq = [sbt(st, f"sq{i}", [128, 512], BF16) for i in range(2)]
                b_sq = fw.bufs(2, "sq")
                rs = [sbt(st, f"rs{i}", [128, 512], F32) for i in range(2)]
                b_rs = fw.bufs(2, "rs")
                sg = [sbt(st, f"sg{i}", [128, 4, 128], BF16) for i in range(2)]
                b_sg = fw.bufs(2, "sg")
                tt = [sbt(st, f"tt{i}", [128, 512], BF16) for i in range(2)]
                b_tt = fw.bufs(2, "tt")
                yb = [sbt(st, f"yb{i}", [128, 8, 512], BF16) for i in range(2)]
                b_yb = fw.bufs(2, "yb", dma=True, stack=st)
                R32 = sbt(st, "R32", [64, 8, 128], F32)
                b_R32 = fw.bufs(2, "R32")
                Rb = [sbt(st, f"Rb{i}", [64, 8, 128], BF16) for i in range(2)]
                b_Rb = [fw.bufs(2, f"Rb{i}_") for i in range(2)]

                fw.dma("sp", b_cB, [lambda h: h.dma_start(out=cdt[:], in_=c_dt),
                                    lambda h: h.dma_start(out=cwq[:], in_=c_wq),
                                    lambda h: h.dma_start(out=cwk[:], in_=c_wk)], writes=[b_cB])
                fw.op("dve", lambda h: h.memset(ones[:], 1.0 / 128), writes=[b_const])
                fw.op("dve", lambda h: h.memset(R32[:], 0.0), writes=b_R32)

                def load_blk(tb):
                    sl = tb % 2
                    cs = slice(tb * 512, (tb + 1) * 512)
                    rs_ = slice(tb * 512, (tb + 1) * 512)
                    fw.dma("sp", b_ld[sl], [
                        lambda h: h.dma_start(out=qTb[sl][:], in_=fm[FM_RQ:FM_RQ + 512, cs].rearrange("(h d) j -> d h j", d=64)),
                        lambda h: h.dma_start(out=kTb[sl][:], in_=fm[FM_RK:FM_RK + 512, cs].rearrange("(h d) j -> d h j", d=64)),
                        lambda h: h.dma_start(out=gTb[sl][:], in_=fm[FM_RG:FM_RG + 1024, cs].rearrange("(h e) j -> e h j", e=128)),
                        lambda h: h.dma_start(out=ktm[sl][:], in_=tm[rs_, TM_RK:TM_RK + 512].rearrange("(u p) n -> p u n", p=128)),
                        lambda h: h.dma_start(out=vtm[sl][:], in_=tm[rs_, TM_RV:TM_RV + 1024].rearrange("(u p) n -> p u n", p=128)),
                    ], reads=[b_fm, b_tm], writes=[b_ld[sl]])

                load_blk(0)

                def ctx(i):
                    n, half = divmod(i, 2)
                    tb, u = divmod(n, 4)
                    return n, half, tb, u, tb % 2, slice(u * 128, (u + 1) * 128), n % 2, i % 2

                def chunk_setup(n):
                    tb, u = divmod(n, 4)
                    sl = tb % 2
                    cs = slice(u * 128, (u + 1) * 128)
                    s = n % 2
                    fw.op("pool", lambda h: h.tensor_tensor(out=qs[s][:], in0=qTb[sl][:, :, cs], in1=cwq[:], op=ALU.mult),
                          reads=[b_ld[sl], b_cB], writes=[b_qs[s]])
                    fw.op("pool", lambda h: h.tensor_tensor(out=kw[s][:], in0=ktm[sl][:, u, :],
                                                            in1=cwk[:].rearrange("p h d -> p (h d)"), op=ALU.mult),
                          reads=[b_ld[sl], b_cB], writes=[b_kw[s]])

                def stage1(i):
                    n, half, tb, u, sl, cs, s, s2 = ctx(i)
                    p_st = s2
                    hs = slice(4 * half, 4 * half + 4)
                    for hh in range(4):
                        hd = 4 * half + hh
                        fw.op("pe", lambda h: h.matmul(ps[p_st][:, hh * 128:(hh + 1) * 128], lhsT=kTb[sl][:, hd, cs],
                                                       rhs=qTb[sl][:, hd, cs], start=True, stop=True),
                              reads=[b_ld[sl]], writes=[b_ps[p_st]])
                    fw.op("dve", lambda h: h.tensor_tensor(out=A[s2][:], in0=ps[p_st][:].rearrange("p (h i) -> p h i", h=4),
                                                           in1=cdt[:, hs, :], op=ALU.mult),
                          reads=[b_ps[p_st], b_cB], writes=[b_A[s2]])

                def stage2(i):
                    n, half, tb, u, sl, cs, s, s2 = ctx(i)
                    rsl = n % 2
                    p_o, p_kv = 2 + i % 3, 5
                    hs = slice(4 * half, 4 * half + 4)
                    for hh in range(4):
                        hd = 4 * half + hh
                        fw.op("pe", lambda h: h.matmul(ps[p_o][:, hh * 128:(hh + 1) * 128],
                                                       lhsT=vtm[sl][:, u, hd * 128:(hd + 1) * 128], rhs=A[s2][:, hh, :],
                                                       start=True, stop=(n == 0)),
                              reads=[b_ld[sl], b_A[s2]], writes=[b_ps[p_o]])
                        if n > 0:
                            fw.op("pe", lambda h: h.matmul(ps[p_o][:, hh * 128:(hh + 1) * 128],
                                                           lhsT=Rb[rsl][:, hd, :], rhs=qs[s][:, hd, :], start=False, stop=True),
                                  reads=[b_Rb[rsl][half], b_qs[s]], writes=[b_ps[p_o]])
                    if n + 1 < NT:
                        for hh in range(4):
                            hd = 4 * half + hh
                            fw.op("pe", lambda h: h.matmul(ps[p_kv][0:64, hh * 128:(hh + 1) * 128],
                                                           lhsT=kw[s][:, hd * 64:(hd + 1) * 64],
                                                           rhs=vtm[sl][:, u, hd * 128:(hd + 1) * 128], start=True, stop=True),
                                  reads=[b_kw[s], b_ld[sl]], writes=[b_ps[p_kv]])
                    fw.op("act", lambda h: h.activation(out=sq[s2][:], in_=ps[p_o][:], func=AF.Square),
                          reads=[b_ps[p_o]], writes=[b_sq[s2]])
                    fw.op("act", lambda h: h.activation(out=sg[s2][:], in_=gTb[sl][:, hs, cs], func=AF.Silu),
                          reads=[b_ld[sl]], writes=[b_sg[s2]])
                    if n + 1 < NT:
                        for hh in range(4):
                            hd = 4 * half + hh
                            fw.op("dve", lambda h: h.scalar_tensor_tensor(out=R32[:, hd, :], in0=R32[:, hd, :], scalar=G128[hd],
                                                                          in1=ps[p_kv][0:64, hh * 128:(hh + 1) * 128],
                                                                          op0=ALU.mult, op1=ALU.add),
                                  reads=[b_ps[p_kv], b_R32[half]], writes=[b_R32[half]])
                        fw.op("act", lambda h: h.activation(out=Rb[1 - rsl][:, hs, :], in_=R32[:, hs, :], func=AF.Copy),
                              reads=[b_R32[half]], writes=[b_Rb[1 - rsl][half]])

                def stage3(i):
                    n, half, tb, u, sl, cs, s, s2 = ctx(i)
                    p_o, p_ss = 2 + i % 3, 6 + s2
                    hs = slice(4 * half, 4 * half + 4)
                    ys = tb % 2
                    fw.op("pe", lambda h: h.matmul(ps[p_ss][:], lhsT=ones[:], rhs=sq[s2][:], start=True, stop=True),
                          reads=[b_sq[s2], b_const], writes=[b_ps[p_ss]])
                    fw.op("act", lambda h: h.activation(out=rs[s2][:], in_=ps[p_ss][:], func=AF.Sqrt, bias=EPS, scale=1.0),
                          reads=[b_ps[p_ss]], writes=[b_rs[s2]])
                    fw.op("dve", lambda h: h.reciprocal(out=rs[s2][:], in_=rs[s2][:]), reads=[b_rs[s2]], writes=[b_rs[s2]])
                    fw.op("dve", lambda h: h.tensor_tensor(out=tt[s2][:], in0=ps[p_o][:], in1=rs[s2][:], op=ALU.mult),
                          reads=[b_ps[p_o], b_rs[s2]], writes=[b_tt[s2]])
                    fw.op("pool", lambda h: h.tensor_tensor(out=yb[ys][:, hs, cs], in0=tt[s2][:].rearrange("p (h i) -> p h i", h=4),
                                                            in1=sg[s2][:], op=ALU.mult),
                          reads=[b_tt[s2], b_sg[s2]], writes=[b_yb[ys]])
                    if u == 3 and half == 1:
                        dst = retT[:, tb * 512:(tb + 1) * 512].rearrange("(h e) j -> e h j", e=128)
                        fw.dma("sp", b_yb[ys], [lambda h: h.dma_start(out=dst, in_=yb[ys][:])], reads=[b_yb[ys]], writes=[b_retT])

                NI = 2 * NT
                chunk_setup(0)
                stage1(0)
                for i in range(NI + 1):
                    if i + 1 < NI:
                        if (i + 1) % 2 == 0:
                            chunk_setup((i + 1) // 2)
                        stage1(i + 1)
                    if i < NI:
                        stage2(i)
                    if i - 1 >= 0:
                        stage3(i - 1)
                    if i % 8 == 0 and i // 8 + 1 < NB5:
                        load_blk(i // 8 + 1)

        if "C" in phases:
            fw.barrier()
            with ExitStack() as st0:
                ebf = sbt(st0, "ebf", [128, 12, 2, 128], F32)
                eb = sbt(st0, "eb", [128, 12, 2, 128], BF16)
                b_eb = fw.buf("eb", dma=True, stack=st0)
                fw.dma("sp", b_eb, [lambda h: h.dma_start(out=ebf[:], in_=c_eb)], writes=[b_eb])
                fw.op("dve", lambda h: h.tensor_copy(out=eb[:], in_=ebf[:]), reads=[b_eb], writes=[b_eb])
                E = [sbt(st0, f"E{i}", [128, 4, 256], BF16) for i in range(2)]
                b_E = fw.bufs(2, "E")
                P = [sbt(st0, f"P{i}", [128, 4, 256], BF16) for i in range(2)]
                b_P = fw.bufs(2, "P")
                osb = [sbt(st0, f"osb{i}", [128, 4, 132], F32) for i in range(2)]
                b_osb = fw.bufs(2, "osb", dma=True, stack=st0)
                b_osbj = [fw.bufs(2, f"osbj{i}_") for i in range(2)]
                for i in range(2):
                    fw.op("dve", lambda h: h.memset(osb[i][:], 0.0), writes=[b_osb[i]] + b_osbj[i])
                scale = 128.0 ** -0.5
                it = 0
                for g, dil in enumerate((1, 4, 16)):
                    SB = 128 * dil
                    nsb = S // SB
                    fw.barrier()
                    with ExitStack() as st:
                        NS = min(3, nsb)
                        qTs = [sbt(st, f"qTs{g}_{i}", [128, 4, SB], BF16) for i in range(2)]
                        b_qTs = fw.bufs(2, "qTs", dma=True, stack=st)
                        kTs = [sbt(st, f"kTs{g}_{i}", [128, 4, SB], BF16) for i in range(NS)]
                        Vs = [sbt(st, f"Vs{g}_{i}", [128, dil, 4, 130], BF16) for i in range(NS)]
                        b_kv = fw.bufs(NS, "kvs", dma=True, stack=st)
                        for i in range(NS):
                            fw.op("dve", lambda h: h.memset(Vs[i][:, :, :, 128:130], 1.0), writes=[b_kv[i]])

                        wide = (dil == 1)
                        if wide:
                            NTW = S // 512
                            qw = [sbt(st, f"qw{i}", [128, 4, 512], BF16) for i in range(2)]
                            kwd = [sbt(st, f"kwd{i}", [128, 4, 512], BF16) for i in range(3)]
                            b_qw = fw.bufs(2, "qw", dma=True, stack=st)
                            b_kwd = fw.bufs(3, "kwd", dma=True, stack=st)

                            def load_wide(T):
                                csw = slice(T * 512, (T + 1) * 512)
                                fw.dma("sp", b_qw[T % 2], [lambda h: h.dma_start(
                                    out=qw[T % 2][:], in_=fm[FM_AQ + g * 512:FM_AQ + (g + 1) * 512, csw].rearrange("(h d) j -> d h j", d=128))],
                                    reads=[b_fm], writes=[b_qw[T % 2]])
                                fw.dma("sp", b_kwd[T % 3], [lambda h: h.dma_start(
                                    out=kwd[T % 3][:], in_=fm[FM_AK + g * 512:FM_AK + (g + 1) * 512, csw].rearrange("(h d) j -> d h j", d=128))],
                                    reads=[b_fm], writes=[b_kwd[T % 3]])

                        def q_op(n, hh, r):
                            if wide:
                                T, j = divmod(n, 4)
                                return qw[T % 2][:, hh, j * 128:(j + 1) * 128], b_qw[T % 2]
                            return qTs[n % 2][:, hh, r::dil], b_qTs[n % 2]

                        def k_op(n, hh, r):
                            if wide:
                                T, j = divmod(n, 4)
                                return kwd[T % 3][:, hh, j * 128:(j + 1) * 128], b_kwd[T % 3]
                            return kTs[n % NS][:, hh, r::dil], b_kv[n % NS]

                        def load_sb(n):
                            cs = slice(n * SB, (n + 1) * SB)
                            s2 = n % 2
                            s3 = n % NS
                            vsrc = tm[cs, TM_AV + g * 512:TM_AV + (g + 1) * 512].rearrange("(i r) (h e) -> i r h e", r=dil, h=4)
                            fns = []
                            if not wide:
                                fw.dma("sp", b_qTs[s2], [lambda h: h.dma_start(
                                    out=qTs[s2][:], in_=fm[FM_AQ + g * 512:FM_AQ + (g + 1) * 512, cs].rearrange("(h d) j -> d h j", d=128))],
                                    reads=[b_fm], writes=[b_qTs[s2]])
                                fns.append(lambda h: h.dma_start(
                                    out=kTs[s3][:], in_=fm[FM_AK + g * 512:FM_AK + (g + 1) * 512, cs].rearrange("(h d) j -> d h j", d=128)))
                            for r in range(dil):
                                fns.append(lambda h, r=r: h.dma_start(out=Vs[s3][:, r, :, 0:128], in_=vsrc[:, r]))
                            fw.dma("sp", b_kv[s3], fns, reads=[b_fm, b_tm], writes=[b_kv[s3]])

                        if wide:
                            load_wide(0)
                        load_sb(0)
                        if nsb > 1:
                            load_sb(1)
                        for n in range(nsb):
                            if wide and n % 4 == 0 and n // 4 + 1 < NTW:
                                load_wide(n // 4 + 1)
                            s2 = n % 2
                            cur = n % NS
                            prv = (n - 1) % NS
                            kprev = prv if n > 0 else cur
                            nprev = n - 1 if n > 0 else n
                            for r in range(dil):
                                w = it % 2
                                it += 1
                                p_s = [w * 2, w * 2 + 1]
                                p_o = [4 + w * 2, 4 + w * 2 + 1]
                                for hh in range(4):
                                    pb = p_s[hh // 2]
                                    o0 = (hh % 2) * 256
                                    q_ap, q_tok = q_op(n, hh, r)
                                    kp_ap, kp_tok = k_op(nprev, hh, r)
                                    kc_ap, kc_tok = k_op(n, hh, r)
                                    fw.op("pe", lambda h: h.matmul(ps[pb][:, o0:o0 + 128], lhsT=kp_ap, rhs=q_ap, start=True, stop=True),
                                          reads=[kp_tok, q_tok], writes=[b_ps[pb]])
                                    fw.op("pe", lambda h: h.matmul(ps[pb][:, o0 + 128:o0 + 256], lhsT=kc_ap, rhs=q_ap, start=True, stop=True),
                                          reads=[kc_tok, q_tok], writes=[b_ps[pb]])
                                for j in range(2):
                                    fw.op("act", lambda h: h.activation(out=E[w][:, 2 * j:2 * j + 2, :].rearrange("p a b -> p (a b)"),
                                                                        in_=ps[p_s[j]][:], func=AF.Exp, scale=scale),
                                          reads=[b_ps[p_s[j]]], writes=[b_E[w]])
                                fw.op("dve", lambda h: h.tensor_tensor(out=P[w][:], in0=E[w][:],
                                                                       in1=eb[:, 4 * g:4 * g + 4, :, :].rearrange("p h t q -> p h (t q)"),
                                                                       op=ALU.mult),
                                      reads=[b_E[w], b_eb], writes=[b_P[w]])
                                for hh in range(4):
                                    pb = p_o[hh // 2]
                                    o0 = (hh % 2) * 256
                                    if n > 0:
                                        fw.op("pe", lambda h: h.matmul(ps[pb][:, o0:o0 + 129], lhsT=P[w][:, hh, 0:128],
                                                                       rhs=Vs[prv][:, r, hh, 0:129], start=True, stop=False),
                                              reads=[b_P[w], b_kv[prv]], writes=[b_ps[pb]])
                                    fw.op("pe", lambda h: h.matmul(ps[pb][:, o0:o0 + 129], lhsT=P[w][:, hh, 128:256],
                                                                   rhs=Vs[cur][:, r, hh, 0:129], start=(n == 0), stop=True),
                                          reads=[b_P[w], b_kv[cur]], writes=[b_ps[pb]])
                                for j in range(2):
                                    evac(osb[w][:, 2 * j:2 * j + 2, 0:129],
                                         ps[p_o[j]][:].rearrange("p (a b) -> p a b", a=2)[:, :, 0:129],
                                         [b_ps[p_o[j]], b_osb[w]], [b_osbj[w][j]])
                                dst = og[g, n * SB:(n + 1) * SB].rearrange("(i r) h e -> i r h e", r=dil)[:, r]
                                fw.dma("sp", b_osb[w], [lambda h: h.dma_start(out=dst, in_=osb[w][:])], reads=b_osbj[w], writes=[b_og, b_osb[w]])
                            if n + 2 < nsb and NS == 3:
                                load_sb(n + 2)

        if "D" in phases:
            fw.barrier()
            with ExitStack() as st:
                Wret = sbt(st, "Wret", [128, 8, D], BF16)
                Watt = sbt(st, "Watt", [128, 4, D], BF16)
                Wout = sbt(st, "Wout", [128, 8, D], BF16)
                g2 = sbt(st, "g2", [128, D], F32)
                b_W = fw.buf("WD", dma=True, stack=st, sw=True)
                fw.dma("pool", b_W, [
                    lambda h: h.dma_start(out=Wret[:], in_=w_ret.rearrange("(c p) n -> p c n", p=128)),
                    lambda h: h.dma_start(out=Watt[:], in_=w_att.rearrange("(c p) n -> p c n", p=128)),
                    lambda h: h.dma_start(out=Wout[:], in_=w_out.rearrange("(c p) n -> p c n", p=128)),
                ], writes=[b_W])
                b_g2 = fw.buf("g2", dma=True, stack=st)
                fw.dma("sp", b_g2, [lambda h: h.dma_start(out=g2[:], in_=g2b)], writes=[b_g2])
                rTb = [sbt(st, f"rTb{i}", [128, 8, 512], BF16) for i in range(2)]
                grb = [sbt(st, f"grb{i}", [128, 8, 512], BF16) for i in range(2)]
                gab = [sbt(st, f"gab{i}", [128, 8, 512], BF16) for i in range(2)]
                b_ldD = fw.bufs(2, "ldD", dma=True, stack=st)
                ogt = [sbt(st, f"ogt{i}", [128, 3, 4, 132], F32) for i in range(2)]
                b_ogt = fw.bufs(2, "ogt", dma=True, stack=st)
                xt = [sbt(st, f"xtD{i}", [128, D], F32) for i in range(2)]
                b_xt = fw.bufs(2, "xtD", dma=True, stack=st)
                acc = sbt(st, "acc", [128, 4, 132], F32)
                b_acc = fw.buf("acc")
                rden = sbt(st, "rden", [128, 4, 1], F32)
                attn = sbt(st, "attn", [128, 4, 128], BF16)
                b_attn = fw.buf("attn")
                aTb = sbt(st, "aTb", [128, 4, 512], BF16)
                b_aTb = fw.bufs(4, "aTb")
                sgr = [sbt(st, f"sgr{i}", [128, 512], BF16) for i in range(2)]
                sga = [sbt(st, f"sga{i}", [128, 512], BF16) for i in range(2)]
                b_sgg = fw.bufs(2, "sgg")
                m1 = [sbt(st, f"m1{i}", [128, 512], F32) for i in range(2)]
                m2 = [sbt(st, f"m2{i}", [128, 512], F32) for i in range(2)]
                b_m = fw.bufs(2, "m")
                mT = sbt(st, "mT", [128, 8, 512], BF16)
                b_mT = fw.bufs(8, "mT")
                hh_ = [sbt(st, f"hD{i}", [128, D], F32) for i in range(2)]
                b_hh = fw.bufs(2, "hD", dma=True, stack=st)
                junk = sbt(st, "junkD", [128, D], BF16)
                b_junk = fw.buf("junkD")
                ssD = sbt(st, "ssD", [128, NT], F32)
                rsD = sbt(st, "rsD", [128, NT], F32)
                b_ssD = fw.bufs(NT, "ssD")
                xn = [sbt(st, f"xnD{i}", [128, D], BF16) for i in range(2)]
                b_xn = fw.bufs(2, "xnD")
                xnTb = [sbt(st, f"xnTb{i}", [128, 8, 512], BF16) for i in range(2)]
                b_xnTb = fw.bufs(2, "xnTb", dma=True, stack=st)

                def load_blkD(tb):
                    sl = tb % 2
                    cs = slice(tb * 512, (tb + 1) * 512)
                    fw.dma("sp", b_ldD[sl], [
                        lambda h: h.dma_start(out=rTb[sl][:], in_=retT[:, cs].rearrange("(c p) j -> p c j", p=128)),
                        lambda h: h.dma_start(out=grb[sl][:], in_=fm[FM_GR:FM_GR + 1024, cs].rearrange("(c p) j -> p c j", p=128)),
                        lambda h: h.dma_start(out=gab[sl][:], in_=fm[FM_GA:FM_GA + 1024, cs].rearrange("(c p) j -> p c j", p=128)),
                    ], reads=[b_fm, b_retT], writes=[b_ldD[sl]])

                def load_og(t):
                    s2 = t % 2
                    rows = slice(t * 128, (t + 1) * 128)
                    fw.dma("sp", b_ogt[s2], [(lambda h, g=g: h.dma_start(out=ogt[s2][:, g], in_=og[g, rows])) for g in range(3)],
                           reads=[b_og], writes=[b_ogt[s2]])

                def load_x(t):
                    s2 = t % 2
                    rows = slice(t * 128, (t + 1) * 128)
                    fw.dma("sp", b_xt[s2], [lambda h: h.dma_start(out=xt[s2][:], in_=x[rows, :])], writes=[b_xt[s2]])

                load_blkD(0)
                load_og(0)
                load_x(0)
                pc = 0
                pcD = [1000]
                for tb in range(NB5):
                    sl = tb % 2
                    if tb + 1 < NB5:
                        load_blkD(tb + 1)
                    for u in range(4):
                        t = tb * 4 + u
                        s2 = t % 2
                        if t + 1 < NT:
                            load_og(t + 1)
                        fw.op("dve", lambda h: h.tensor_tensor(out=acc[:], in0=ogt[s2][:, 0], in1=ogt[s2][:, 1], op=ALU.add),
                              reads=[b_ogt[s2]], writes=[b_acc])
                        fw.op("dve", lambda h: h.tensor_tensor(out=acc[:], in0=acc[:], in1=ogt[s2][:, 2], op=ALU.add),
                              reads=[b_ogt[s2], b_acc], writes=[b_acc])
                        fw.op("dve", lambda h: h.reciprocal(out=rden[:], in_=acc[:, :, 128:129]), reads=[b_acc], writes=[b_acc])
                        fw.op("dve", lambda h: h.tensor_tensor(out=attn[:], in0=acc[:, :, 0:128], in1=rden[:].to_broadcast([128, 4, 128]),
                                                               op=ALU.mult), reads=[b_acc], writes=[b_attn])
                        pb = pc % 2
                        pc += 1
                        psb = ps[pb][:].bitcast(BF16)
                        for c in range(4):
                            fw.op("pe", lambda h: h.transpose(out=psb[:, c * 128:(c + 1) * 128], in_=attn[:, c, :], identity=ident[:]),
                                  reads=[b_attn, b_const], writes=[b_ps[pb]])
                        evac(aTb[:, :, u * 128:(u + 1) * 128], psb[:, 0:512].rearrange("p (c j) -> p c j", c=4), [b_ps[pb]], [b_aTb[u]])
                        if t + 1 < NT:
                            pass
                    for ncn in range(8):
                        w2 = ncn % 2
                        p_r, p_a = 2 + w2, 4 + w2
                        for c in range(8):
                            fw.op("pe", lambda h: h.matmul(ps[p_r][:], lhsT=Wret[:, c, ncn * 128:(ncn + 1) * 128], rhs=rTb[sl][:, c, :],
                                                           start=(c == 0), stop=(c == 7)),
                                  reads=[b_W, b_ldD[sl]], writes=[b_ps[p_r]], inc=(c == 7))
                        for c in range(4):
                            fw.op("pe", lambda h: h.matmul(ps[p_a][:], lhsT=Watt[:, c, ncn * 128:(ncn + 1) * 128], rhs=aTb[:, c, :],
                                                           start=(c == 0), stop=(c == 3)),
                                  reads=[b_W] + b_aTb, writes=[b_ps[p_a]], inc=(c == 3))
                        fw.op("act", lambda h: h.activation(out=sgr[w2][:], in_=grb[sl][:, ncn, :], func=AF.Sigmoid),
                              reads=[b_ldD[sl]], writes=[b_sgg[w2]])
                        fw.op("act", lambda h: h.activation(out=sga[w2][:], in_=gab[sl][:, ncn, :], func=AF.Sigmoid),
                              reads=[b_ldD[sl]], writes=[b_sgg[w2]])
                        fw.op("dve", lambda h: h.tensor_tensor(out=m1[w2][:], in0=ps[p_r][:], in1=sgr[w2][:], op=ALU.mult),
                              reads=[b_ps[p_r], b_sgg[w2]], writes=[b_m[w2]])
                        fw.op("dve", lambda h: h.tensor_tensor(out=m2[w2][:], in0=ps[p_a][:], in1=sga[w2][:], op=ALU.mult),
                              reads=[b_ps[p_a], b_sgg[w2]], writes=[b_m[w2]])
                        fw.op("dve", lambda h: h.tensor_tensor(out=mT[:, ncn, :], in0=m1[w2][:], in1=m2[w2][:], op=ALU.add),
                              reads=[b_m[w2]], writes=[b_mT[ncn]])
                    p_h = [6, 7]

                    def wout_mm(u):
                        t = tb * 4 + u
                        s2 = t % 2
                        for half in range(2):
                            for c in range(8):
                                fw.op("pe", lambda h: h.matmul(ps[p_h[half]][:], lhsT=mT[:, c, u * 128:(u + 1) * 128],
                                                               rhs=Wout[:, c, half * 512:(half + 1) * 512], start=(c == 0), stop=(c == 7)),
                                      reads=[b_W] + b_mT, writes=[b_ps[p_h[half]]], inc=(c == 7))
                        for half in range(2):
                            fw.op("dve", lambda h: h.tensor_tensor(out=hh_[s2][:, half * 512:(half + 1) * 512], in0=ps[p_h[half]][:],
                                                                   in1=xt[s2][:, half * 512:(half + 1) * 512], op=ALU.add),
                                  reads=[b_ps[p_h[half]], b_xt[s2]], writes=[b_hh[s2]])
                        fw.dma("sp", b_hh[s2], [lambda h: h.dma_start(out=hscr[t * 128:(t + 1) * 128, :], in_=hh_[s2][:])],
                               reads=[b_hh[s2]], writes=[b_h])
                        rms_rstd(None, ssD[:, t:t + 1], rsD[:, t:t + 1], b_ssD[t], hh_[s2][:], junk[:], b_hh[s2], b_junk, D)
                        fw.op("dve", lambda h: h.scalar_tensor_tensor(out=xn[s2][:], in0=hh_[s2][:], scalar=rsD[:, t:t + 1], in1=g2[:],
                                                                      op0=ALU.mult, op1=ALU.mult),
                              reads=[b_hh[s2], b_ssD[t], b_g2], writes=[b_xn[s2]])
                        if t + 1 < NT:
                            load_x(t + 1)

                    def wout_tr(u):
                        nonlocal_pc = pcD[0]
                        pcD[0] += 1
                        t = tb * 4 + u
                        s2 = t % 2
                        pb = nonlocal_pc % 2
                        psb = ps[pb][:].bitcast(BF16)
                        for c in range(8):
                            fw.op("pe", lambda h: h.transpose(out=psb[:, c * 128:(c + 1) * 128], in_=xn[s2][:, c * 128:(c + 1) * 128],
                                                              identity=ident[:]),
                                  reads=[b_xn[s2], b_const], writes=[b_ps[pb]])
                        evac(xnTb[sl][:, :, u * 128:(u + 1) * 128], psb.rearrange("p (c j) -> p c j", c=8), [b_ps[pb]], [b_xnTb[sl]])

                    wout_mm(0)
                    for u in range(4):
                        if u + 1 < 4:
                            wout_mm(u + 1)
                        wout_tr(u)
                    dst = xn2T[:, tb * 512:(tb + 1) * 512].rearrange("(c p) j -> p c j", p=128)
                    fw.dma("sp", b_xnTb[sl], [lambda h: h.dma_start(out=dst, in_=xnTb[sl][:])], reads=[b_xnTb[sl]], writes=[b_xn2T])

        if "E" in phases:
            fw.barrier()
            with ExitStack() as st:
                Wq = sbt(st, "Wq", [128, 8, 2048], BF16)
                skb = sbt(st, "skb", [128, 16, 128], BF16)
                b_WE = fw.buf("WE", dma=True, stack=st, sw=True)
                fw.dma("pool", b_WE, [
                    lambda h: h.dma_start(out=Wq[:], in_=wq.rearrange("(c p) n -> p c n", p=128)),
                    lambda h: h.dma_start(out=skb[:], in_=skT),
                ], writes=[b_WE])
                xnb = [sbt(st, f"xnbE{i}", [128, 8, 512], BF16) for i in range(2)]
                b_xnb = fw.bufs(2, "xnbE", dma=True, stack=st)
                qTb2 = [sbt(st, f"qTbE{i}", [128, 16, 512], BF16) for i in range(2)]
                b_qTb2 = [fw.bufs(16, f"qTbE{i}_") for i in range(2)]
                sc = [sbt(st, f"sc{i}", [128, 16, 128], F32) for i in range(2)]
                b_sc = [fw.bufs(4, f"sc{i}_") for i in range(2)]
                work = sbt(st, "workE", [128, 16, 128], F32)
                b_work = fw.bufs(16, "workE")
                tops = sbt(st, "tops", [128, 16, 16], F32)
                idxs = sbt(st, "idxs", [128, 16, 16], U32)
                b_top = fw.bufs(16, "top")
                idxf = sbt(st, "idxf", [128, 16, 16], BF16)
                b_idxf = fw.buf("idxf")
                cand = sbt(st, "cand", [128, 8, 256], F32)
                b_cand = fw.buf("cand")
                work2 = sbt(st, "work2", [128, 8, 256], F32)
                b_work2 = fw.bufs(8, "work2")
                bests = sbt(st, "bests", [128, 8, 16], F32)
                pos = sbt(st, "pos", [128, 8, 16], U32)
                b_best = fw.bufs(8, "best")
                aru = sbt(st, "aru", [128, 8, 16], U32)
                bru = sbt(st, "bru", [128, 8, 16], U32)
                brf = sbt(st, "brf", [128, 8, 16], BF16)
                arf = sbt(st, "arf", [128, 8, 16], BF16)
                b_pf = fw.buf("pf")
                oha = sbt(st, "oha", [128, 8, 16, 16], BF16)
                ohb = sbt(st, "ohb", [128, 8, 16, 16], BF16)
                b_oha = fw.buf("oha")
                b_ohb = fw.buf("ohb")
                sel3 = sbt(st, "sel3", [128, 3, 128], BF16)
                b_sel3 = fw.bufs(3, "sel3")
                ee = sbt(st, "ee", [128, 8, 16], F32)
                esum = sbt(st, "esum", [128, 8, 1], F32)
                b_ee = fw.buf("ee")
                selT = [sbt(st, f"selT{i}", [128, 3, 512], BF16) for i in range(2)]
                b_selT = fw.bufs(2, "selT", dma=True, stack=st)
                sel_dst = (iaT, ibT, GT)

                def load_xnb(tb):
                    sl = tb % 2
                    fw.dma("sp", b_xnb[sl], [lambda h: h.dma_start(
                        out=xnb[sl][:], in_=xn2T[:, tb * 512:(tb + 1) * 512].rearrange("(c p) j -> p c j", p=128))],
                        reads=[b_xn2T], writes=[b_xnb[sl]])

                load_xnb(0)
                pc = 0
                iota16b = sbt(st, "iota16b", [128, 16], BF16)
                fw.op("dve", lambda h: h.tensor_copy(out=iota16b[:], in_=iota_f[:, 0:16]), reads=[b_const], writes=[b_const])
                iota16 = iota16b[:]
                npay = sbt(st, "npay", [128, 128], F32)
                fw.op("dve", lambda h: h.tensor_single_scalar(out=npay[:], in_=iota_f[:], scalar=2.0 ** -18, op=ALU.mult),
                      reads=[b_const], writes=[b_const])
                thr16 = sbt(st, "thr16", [128, 16], F32)
                fw.op("dve", lambda h: h.tensor_scalar(out=thr16[:], in0=iota16, scalar1=16.0, scalar2=16.0, op0=ALU.mult, op1=ALU.add),
                      reads=[b_const], writes=[b_const])
                qpc = [0]

                def emit_q(tbq, hps):
                    slq = tbq % 2
                    for hp in hps:
                        pb = qpc[0] % 4
                        qpc[0] += 1
                        for c in range(8):
                            fw.op("pe", lambda h: h.matmul(ps[pb][:], lhsT=Wq[:, c, hp * 128:(hp + 1) * 128], rhs=xnb[slq][:, c, :],
                                                           start=(c == 0), stop=(c == 7)),
                                  reads=[b_WE, b_xnb[slq]], writes=[b_ps[pb]], inc=(c == 7))
                        fw.op("act", lambda h: h.activation(out=qTb2[slq][:, hp, :], in_=ps[pb][:], func=AF.Copy),
                              reads=[b_ps[pb]], writes=[b_qTb2[slq][hp]])

                def emit_scores(t):
                    tb_, u_ = divmod(t, 4)
                    s2_ = t % 2
                    if "F" in phases and t < 16:
                        conv_piece(t)
                    if u_ == 0 and tb_ + 1 < NB5:
                        load_xnb(tb_ + 1)
                    if tb_ + 1 < NB5:
                        emit_q(tb_ + 1, range(4 * u_, 4 * u_ + 4))
                    for j in range(4):
                        for k in range(4):
                            hp = 4 * j + k
                            fw.op("pe", lambda h: h.matmul(ps[4 + j][:, k * 128:(k + 1) * 128], lhsT=qTb2[tb_ % 2][:, hp, u_ * 128:(u_ + 1) * 128],
                                                           rhs=skb[:, hp, :], start=True, stop=True),
                                  reads=[b_qTb2[tb_ % 2][hp], b_WE], writes=[b_ps[4 + j]])
                        fw.op("act", lambda h: h.activation(out=sc[s2_][:, 4 * j:4 * j + 4, :], in_=ps[4 + j][:].rearrange("p (k n) -> p k n", k=4),
                                                            func=AF.Identity, bias=6144.0, scale=1.0), reads=[b_ps[4 + j]], writes=[b_sc[s2_][j]])

                emit_q(0, range(16))
                emit_scores(0)
                for tb in range(NB5):
                    sl = tb % 2
                    for u in range(4):
                        t = tb * 4 + u
                        s2 = t % 2
                        if t + 1 < NT:
                            emit_scores(t + 1)
                        fw.op("dve", lambda h: h.scalar_tensor_tensor(out=sc[s2][:], in0=sc[s2][:], scalar=-6096.0,
                                                                      in1=npay[:].unsqueeze(1).to_broadcast([128, 16, 128]),
                                                                      op0=ALU.add, op1=ALU.add),
                              reads=b_sc[s2] + [b_const], writes=b_sc[s2])
                        for hp in range(16):
                            fw.op("dve", lambda h: h.max(out=tops[:, hp, 0:8], in_=sc[s2][:, hp, :]),
                                  reads=[b_sc[s2][hp // 4]], writes=[b_top[hp]])
                        for hp in range(16):
                            fw.op("dve", lambda h: h.match_replace(out=work[:, hp, :], in_to_replace=tops[:, hp, 0:8],
                                                                   in_values=sc[s2][:, hp, :], imm_value=NEG),
                                  reads=[b_sc[s2][hp // 4], b_top[hp]], writes=[b_work[hp]])
                        for hp in range(16):
                            fw.op("dve", lambda h: h.max(out=tops[:, hp, 8:16], in_=work[:, hp, :]),
                                  reads=[b_work[hp]], writes=[b_top[hp]])
                        fw.op("dve", lambda h: h.tensor_single_scalar(out=idxs[:], in_=tops[:].bitcast(U32), scalar=127, op=ALU.bitwise_and),
                              reads=b_top, writes=b_top)
                        fw.op("dve", lambda h: h.tensor_copy(out=idxf[:], in_=idxs[:]), reads=b_top, writes=[b_idxf])
                        tops4 = tops[:].rearrange("p (h two) k -> p h two k", two=2)
                        idxf4 = idxf[:].rearrange("p (h two) k -> p h two k", two=2)
                        fw.op("dve", lambda h: h.tensor_tensor(out=cand[:].rearrange("p h (a b) -> p h a b", a=16),
                                                               in0=tops4[:, :, 0, :].unsqueeze(3).to_broadcast([128, 8, 16, 16]),
                                                               in1=tops4[:, :, 1, :].unsqueeze(2).to_broadcast([128, 8, 16, 16]), op=ALU.add),
                              reads=b_top, writes=[b_cand])
                        for hd in range(8):
                            fw.op("dve", lambda h: h.max(out=bests[:, hd, 0:8], in_=cand[:, hd, :]), reads=[b_cand], writes=[b_best[hd]])
                        for hd in range(8):
                            fw.op("dve", lambda h: h.max_index(out=pos[:, hd, 0:8], in_max=bests[:, hd, 0:8], in_values=cand[:, hd, :]),
                                  reads=[b_cand], writes=[b_best[hd]])
                        for hd in range(8):
                            fw.op("dve", lambda h: h.match_replace(out=work2[:, hd, :], in_to_replace=bests[:, hd, 0:8],
                                                                   in_values=cand[:, hd, :], imm_value=NEG),
                                  reads=[b_cand, b_best[hd]], writes=[b_work2[hd]])
                        for hd in range(8):
                            fw.op("dve", lambda h: h.max(out=bests[:, hd, 8:16], in_=work2[:, hd, :]), reads=[b_work2[hd]], writes=[b_best[hd]])
                        for hd in range(8):
                            fw.op("dve", lambda h: h.max_index(out=pos[:, hd, 8:16], in_max=bests[:, hd, 8:16], in_values=work2[:, hd, :]),
                                  reads=[b_work2[hd]], writes=[b_best[hd]])
                        fw.op("dve", lambda h: h.tensor_single_scalar(out=aru[:], in_=pos[:], scalar=4, op=ALU.logical_shift_right),
                              reads=b_best, writes=[b_pf])
                        fw.op("dve", lambda h: h.tensor_single_scalar(out=bru[:], in_=pos[:], scalar=15, op=ALU.bitwise_and),
                              reads=b_best, writes=[b_pf])
                        fw.op("dve", lambda h: h.tensor_copy(out=arf[:], in_=aru[:]), reads=[b_pf], writes=[b_pf])
                        fw.op("dve", lambda h: h.tensor_copy(out=brf[:], in_=bru[:]), reads=[b_pf], writes=[b_pf])
                        io4 = iota16.unsqueeze(1).unsqueeze(1).to_broadcast([128, 8, 16, 16])
                        for which, (oh, b_oh, rk) in enumerate(((oha, b_oha, arf), (ohb, b_ohb, brf))):
                            fw.op("dve", lambda h: h.tensor_tensor(out=oh[:], in0=io4, in1=rk[:].unsqueeze(3).to_broadcast([128, 8, 16, 16]),
                                                                 op=ALU.is_equal), reads=[b_pf, b_const], writes=[b_oh])
                            fw.op("dve", lambda h: h.tensor_tensor(out=oh[:], in0=oh[:],
                                                                 in1=idxf4[:, :, which, :].unsqueeze(2).to_broadcast([128, 8, 16, 16]),
                                                                 op=ALU.mult), reads=[b_oh, b_idxf], writes=[b_oh])
                            fw.op("dve", lambda h: h.tensor_reduce(out=sel3[:, which, :].rearrange("p (h k) -> p h k", h=8), in_=oh[:],
                                                                   axis=AX.X, op=ALU.add), reads=[b_oh], writes=[b_sel3[which]])
                        fw.op("dve", lambda h: h.tensor_tensor(out=ee[:], in0=bests[:], in1=bests[:, :, 0:1].to_broadcast([128, 8, 16]),
                                                               op=ALU.subtract), reads=b_best, writes=[b_ee])
                        fw.op("act", lambda h: h.activation(out=ee[:], in_=ee[:], func=AF.Exp), reads=[b_ee], writes=[b_ee])
                        fw.op("dve", lambda h: h.tensor_reduce(out=esum[:], in_=ee[:], axis=AX.X, op=ALU.add), reads=[b_ee], writes=[b_ee])
                        fw.op("dve", lambda h: h.reciprocal(out=esum[:], in_=esum[:]), reads=[b_ee], writes=[b_ee])
                        fw.op("dve", lambda h: h.tensor_tensor(out=sel3[:, 2, :].rearrange("p (h k) -> p h k", h=8), in0=ee[:],
                                                               in1=esum[:].to_broadcast([128, 8, 16]), op=ALU.mult),
                              reads=[b_ee], writes=[b_sel3[2]])
                        for k3 in range(3):
                            pb = qpc[0] % 4
                            qpc[0] += 1
                            psb = ps[pb][:].bitcast(BF16)
                            fw.op("pe", lambda h: h.transpose(out=psb[:, 0:128], in_=sel3[:, k3, :], identity=ident[:]),
                                  reads=[b_sel3[k3], b_const], writes=[b_ps[pb]])
                            fw.op("act", lambda h: h.activation(out=selT[sl][:, k3, u * 128:(u + 1) * 128], in_=psb[:, 0:128], func=AF.Copy),
                                  reads=[b_ps[pb]], writes=[b_selT[sl]])
                    fw.dma("sp", b_selT[sl], [(lambda h, k3=k3: h.dma_start(out=sel_dst[k3][:, tb * 512:(tb + 1) * 512], in_=selT[sl][:, k3, :]))
                                              for k3 in range(3)], reads=[b_selT[sl]], writes=[b_sel])

        if "F" in phases:
            fw.barrier()
            with ExitStack() as st:
                TB = 256
                NBK = S // TB
                Wall = [sbt(st, f"Wall{i}", [128, TB, 128], BF16) for i in range(2)]
                b_Wall = fw.bufs(2, "Wall")
                NUV = 4
                Uc = [sbt(st, f"Uc{i}", [128, 2, 8, 128], BF16) for i in range(NUV)]
                Vc = [sbt(st, f"Vc{i}", [128, 2, 1024], BF16) for i in range(NUV)]
                b_Uc = fw.bufs(NUV, "Uc", dma=True, stack=st)
                b_Vc = fw.bufs(NUV, "Vc", dma=True, stack=st)
                hT = sbt(st, "hT", [128, 2, D], F32)
                b_hT = fw.bufs(2, "hT", dma=True, stack=st)
                xnF = [sbt(st, f"xnF{i}", [128, 8, TB], BF16) for i in range(2)]
                selF = [sbt(st, f"selF{i}", [128, 3, TB], BF16) for i in range(2)]
                b_ldF = fw.bufs(2, "ldF", dma=True, stack=st)
                gf = sbt(st, "gf", [128, D], F32)
                b_gf = fw.buf("gf", dma=True, stack=st)
                fw.dma("sp", b_gf, [lambda h: h.dma_start(out=gf[:], in_=gfb)], writes=[b_gf])
                iota_b = sbt(st, "iota_b", [128, 128], BF16)
                fw.op("dve", lambda h: h.tensor_copy(out=iota_b[:], in_=iota_f[:]), reads=[b_const], writes=[b_const])
                NSL = 3
                OA = [sbt(st, f"OA{i}", [128, 4, 128], BF16) for i in range(NSL)]
                OG = [sbt(st, f"OG{i}", [128, 4, 128], BF16) for i in range(NSL)]
                b_OA = fw.bufs(NSL, "OA")
                b_OG = fw.bufs(NSL, "OG")
                b_OGa = fw.bufs(NSL, "OGa")
                selF32 = sbt(st, "selF32", [128, 3, TB], F32)
                negs = sbt(st, "negs", [128, 2, TB], F32)
                tmpA = [sbt(st, f"tmpA{i}", [128, 128], F32) for i in range(2)]
                b_tmpA = fw.bufs(2, "tmpA")
                b_sel32 = fw.buf("sel32")
                Gl = [sbt(st, f"Gl{i}", [128, TB], BF16) for i in range(3)]
                Wg = [sbt(st, f"Wg{i}", [128, TB], BF16) for i in range(3)]
                b_Gl = fw.bufs(3, "Gl")
                b_Wg = fw.bufs(3, "Wg")
                junk = sbt(st, "junkF", [128, D], BF16)
                b_junk = fw.buf("junkF")
                ssF = sbt(st, "ssF", [128, NT], F32)
                rsF = sbt(st, "rsF", [128, NT], F32)
                b_ssF = fw.bufs(NT, "ssF")
                sel_src = (iaT, ibT, GT)

                def load_blkF(bk):
                    sl = bk % 2
                    cs = slice(bk * TB, (bk + 1) * TB)
                    fns = [lambda h: h.dma_start(out=xnF[sl][:], in_=xn2T[:, cs].rearrange("(c p) j -> p c j", p=128))]
                    for k3 in range(3):
                        fns.append(lambda h, k3=k3: h.dma_start(out=selF[sl][:, k3, :], in_=sel_src[k3][:, cs]))
                    fw.dma("sp", b_ldF[sl], fns, reads=[b_xn2T, b_sel], writes=[b_ldF[sl]])

                def load_h(bk):
                    for sub in range(2):
                        rows = slice(bk * TB + sub * 128, bk * TB + (sub + 1) * 128)
                        fw.dma("sp", b_hT[sub], [lambda h: h.dma_start(out=hT[:, sub, :], in_=hscr[rows, :])], reads=[b_h], writes=[b_hT[sub]])

                NG = 64

                def load_uv(gi):
                    ag = gi % NG
                    s4 = gi % NUV
                    rows = slice(ag * 256, (ag + 1) * 256)
                    fw.dma("sp", b_Uc[s4], [lambda h: h.dma_start(out=Uc[s4][:], in_=ubf[rows, :].rearrange("(a p) (c b) -> p a c b", p=128, c=8))],
                           reads=[b_ubf], writes=[b_Uc[s4]])
                    fw.dma("sp", b_Vc[s4], [lambda h: h.dma_start(out=Vc[s4][:], in_=vbf[rows, :].rearrange("(a b) n -> b a n", b=128))],
                           reads=[b_vbf], writes=[b_Vc[s4]])

                def prep_block(bk):
                    sl = bk % 2
                    fw.op("dve", lambda h: h.tensor_copy(out=selF32[:], in_=selF[sl][:]), reads=[b_ldF[sl]], writes=[b_sel32])
                    fw.op("dve", lambda h: h.tensor_single_scalar(out=negs[:], in_=selF32[:, 1:3, :], scalar=-1.0, op=ALU.mult),
                          reads=[b_sel32], writes=[b_sel32])

                osl = [0]
                acnt = [0]

                def build_steps(bk):
                    x = bk % 2
                    NGR = TB // 4
                    slots = {}

                    def onehots(g):
                        w = osl[0] % NSL
                        osl[0] += 1
                        slots[g] = w
                        for tt_ in range(4):
                            t = g * 4 + tt_
                            fw.op("dve", lambda h: h.tensor_scalar(out=OA[w][:, tt_, :], in0=iota_b[:], scalar1=selF32[:, 0, t:t + 1],
                                                                   scalar2=None, op0=ALU.is_equal),
                                  reads=[b_sel32, b_const], writes=[b_OA[w]])
                            if tt_ == 0:
                                ta = acnt[0] % 2
                                acnt[0] += 1
                                fw.op("act", lambda h: h.activation(out=tmpA[ta][:], in_=iota_f[:], func=AF.Square, bias=negs[:, 0, t:t + 1], scale=1.0),
                                      reads=[b_sel32, b_const], writes=[b_tmpA[ta]])
                                fw.op("act", lambda h: h.activation(out=OG[w][:, tt_, :], in_=tmpA[ta][:], func=AF.Relu, bias=selF32[:, 2, t:t + 1],
                                                                    scale=negs[:, 1, t:t + 1]),
                                      reads=[b_sel32, b_tmpA[ta]], writes=[b_OGa[w]])
                            else:
                                fw.op("dve", lambda h: h.tensor_scalar(out=OG[w][:, tt_, :], in0=iota_b[:], scalar1=selF32[:, 1, t:t + 1],
                                                                       scalar2=selF32[:, 2, t:t + 1], op0=ALU.is_equal, op1=ALU.mult),
                                      reads=[b_sel32, b_const], writes=[b_OG[w]])

                    def wmm(g):
                        w = slots[g]
                        pb = 6 + g % 2
                        for tt_ in range(4):
                            fw.op("pe", lambda h: h.matmul(ps[pb][:, tt_ * 128:(tt_ + 1) * 128], lhsT=OG[w][:, tt_, :], rhs=OA[w][:, tt_, :],
                                                           start=True, stop=True), reads=[b_OG[w], b_OGa[w], b_OA[w]], writes=[b_ps[pb]], inc=(tt_ == 3))
                        t4 = slice(g * 4, g * 4 + 4)
                        fw.op("act", lambda h: h.activation(out=Wall[x][:, t4, :], in_=ps[pb][:].rearrange("p (t a) -> p t a", t=4), func=AF.Copy),
                              reads=[b_ps[pb]], writes=[b_Wall[x]])

                    LAG = 1
                    steps = []
                    for g in range(NGR + LAG):
                        fns = []
                        if g - LAG >= 0:
                            fns.append(lambda g=g: wmm(g - LAG))
                        if g < NGR:
                            fns.append(lambda g=g: onehots(g))
                        steps.append(lambda fns=fns: [f() for f in fns])
                    return steps

                load_blkF(0)
                for gi0 in range(NUV - 1):
                    load_uv(gi0)
                prep_block(0)
                for stp in build_steps(0):
                    stp()
                for bk in range(NBK):
                    sl = bk % 2
                    x = bk % 2
                    if bk + 1 < NBK:
                        load_blkF(bk + 1)
                    load_h(bk)
                    pending = []
                    if bk + 1 < NBK:
                        prep_block(bk + 1)
                        pending = build_steps(bk + 1)
                    nsteps = len(pending)
                    done_steps = 0

                    def emit_H(a, s4, aa):
                        e2 = a % 3
                        ph = 4 + a % 2
                        for c in range(8):
                            fw.op("pe", lambda h: h.matmul(ps[ph][:, 0:TB], lhsT=Uc[s4][:, aa, c, :], rhs=xnF[sl][:, c, :],
                                                           start=(c == 0), stop=(c == 7)),
                                  reads=[b_Uc[s4], b_ldF[sl]], writes=[b_ps[ph]], inc=(c == 7))
                        fw.op("act", lambda h: h.activation(out=Gl[e2][:], in_=ps[ph][:, 0:TB], func=AF.Gelu),
                              reads=[b_ps[ph]], writes=[b_Gl[e2]])
                        fw.op("pool", lambda h: h.tensor_tensor(out=Wg[e2][:], in0=Gl[e2][:], in1=Wall[x][:, :, a], op=ALU.mult),
                              reads=[b_Gl[e2], b_Wall[x]], writes=[b_Wg[e2]])

                    def emit_V(a, s4, aa):
                        e2 = a % 3
                        for sub in range(2):
                            for half in range(2):
                                po = sub * 2 + half
                                fw.op("pe", lambda h: h.matmul(ps[po][:], lhsT=Wg[e2][:, sub * 128:(sub + 1) * 128],
                                                               rhs=Vc[s4][:, aa, half * 512:(half + 1) * 512], start=(a == 0), stop=(a == 127)),
                                      reads=[b_Wg[e2], b_Vc[s4]], writes=[b_ps[po]], inc=(po == 3))

                    g0 = bk * NG

                    def uv_slot(a):
                        return (g0 + a // 2) % NUV

                    emit_H(0, uv_slot(0), 0)
                    emit_H(1, uv_slot(1), 1)
                    for a in range(128):
                        ag, aa = divmod(a, 2)
                        if aa == 0 and g0 + ag + NUV - 1 < NBK * NG:
                            load_uv(g0 + ag + NUV - 1)
                        if a + 2 < 128:
                            emit_H(a + 2, uv_slot(a + 2), (a + 2) % 2)
                        want = min(nsteps, (a + 1) * nsteps // 120)
                        while done_steps < want and pending:
                            pending.pop(0)()
                            done_steps += 1
                        emit_V(a, uv_slot(a), aa)
                    while pending:
                        pending.pop(0)()
                    for sub in range(2):
                        t = bk * 2 + sub
                        for half in range(2):
                            po = sub * 2 + half
                            fw.op("dve", lambda h: h.tensor_tensor(out=hT[:, sub, half * 512:(half + 1) * 512], in0=ps[po][:],
                                                                   in1=hT[:, sub, half * 512:(half + 1) * 512], op=ALU.add),
                                  reads=[b_ps[po], b_hT[sub]], writes=[b_hT[sub]])
                        rms_rstd(None, ssF[:, t:t + 1], rsF[:, t:t + 1], b_ssF[t], hT[:, sub, :], junk[:], b_hT[sub], b_junk, D)
                        fw.op("dve", lambda h: h.scalar_tensor_tensor(out=hT[:, sub, :], in0=hT[:, sub, :], scalar=rsF[:, t:t + 1], in1=gf[:],
                                                                      op0=ALU.mult, op1=ALU.mult),
                              reads=[b_ssF[t], b_gf], writes=[b_hT[sub]])
                        fw.dma("sp", b_hT[sub], [lambda h: h.dma_start(out=out[t * 128:(t + 1) * 128, :], in_=hT[:, sub, :])],
                               reads=[b_hT[sub]], writes=[b_out])

        fw.finish([b_fm, b_tm, b_retT, b_og, b_h, b_xn2T, b_sel, b_out, b_ubf, b_vbf])
        nc._fw_ninst = fw.ninst
    return nc


def make_in_maps(inputs, S=4096):
    f = lambda a: np.ascontiguousarray(np.asarray(a, dtype=np.float32))
    x = f(inputs["x"])
    dt_ret, wq_ret, wk_ret, _, eb = _constants()
    rep = lambda v: np.ascontiguousarray(np.broadcast_to(f(v).reshape(1, D), (128, D)))
    u = f(inputs["peer_u"])[0].reshape(128, 128, 8, 128)
    common = {
        "w_in": f(inputs["w_in"])[0],
        "g1b": rep(inputs["norm1_g"][0]),
        "g2b": rep(inputs["norm2_g"][0]),
        "gfb": rep(inputs["normf_g"]),
        "w_ret": f(inputs["w_ret_out"])[0],
        "w_att": f(inputs["w_att_out"])[0],
        "w_out": f(inputs["w_out"])[0],
        "wq": f(inputs["peer_wq"])[0],
        "skT": np.ascontiguousarray(f(inputs["peer_subkeys"])[0].transpose(3, 0, 1, 2).reshape(128, 16, 128)),
        "ut": np.ascontiguousarray(u.transpose(0, 3, 2, 1).reshape(16384, 1024)),
        "vt": f(inputs["peer_v"])[0],
        "c_dt": dt_ret, "c_wq": wq_ret, "c_wk": wk_ret, "c_eb": eb,
    }
    return [dict(common, x=np.ascontiguousarray(x[b, :S])) for b in range(x.shape[0])]


def kernel(**inputs):
    S = 4096
    nc = build_nc(S=S)
    maps = make_in_maps(inputs, S=S)
    res = run_bass_kernel_spmd(nc, maps, core_ids=list(range(8)))
    return np.stack([np.asarray(r["out"], dtype=np.float32) for r in res.results], axis=0)
```
